# Optimizing a Trainium2 kernel written in Bass

```python
import math
import jax, jax.numpy as jnp
from jax import lax
import numpy as np

D_MODEL = 2048
BATCH = 8
SEQ = 2048
DEPTH = 1

HEAD_DIM = 128
A_Q_HEADS = 8
A_KV_HEADS = 2
A_GROUP = A_Q_HEADS // A_KV_HEADS
A_ROPE_THETA = 10000.0
A_WIDTH = A_Q_HEADS * HEAD_DIM
B_HEADS = 4
B_QK_DIM = 128
B_V_DIM = 2 * B_QK_DIM
B_WIDTH = B_HEADS * B_V_DIM
PARTIAL_ROPE_THETA = 500000.0
PARTIAL_ROPE_DIMS = B_QK_DIM // 4
GRID_W = 64
Q_BLOCK = 128
N_EXPERTS = 16
EXPERT_FF = 4096
CAPACITY_FACTOR = 2
RMS_EPS = 1e-6
LN_EPS = 1e-5
DEEPNORM_ALPHA = (2.0 * DEPTH) ** 0.25
DEEPNORM_BETA = (8.0 * DEPTH) ** -0.25
N_BRANCHES = 2
COL_A_Q = A_Q_HEADS * HEAD_DIM
COL_A_K = A_KV_HEADS * HEAD_DIM
COL_A_V = A_KV_HEADS * HEAD_DIM
COL_B_Q = B_HEADS * 2 * B_QK_DIM
COL_B_K = B_HEADS * 2 * B_QK_DIM
COL_B_V = B_HEADS * B_V_DIM
COL_GATES = N_BRANCHES * D_MODEL
IN_COLS = COL_A_Q + COL_A_K + COL_A_V + COL_B_Q + COL_B_K + COL_B_V + COL_GATES

kernel_name = "hybrid_gqa_diffattn_ec_moe_block"


def rms_norm(x, g):
    xf = x.astype(jnp.float32)
    y = xf * lax.rsqrt(jnp.mean(xf * xf, axis=-1, keepdims=True) + RMS_EPS)
    return (y * g.astype(jnp.float32)).astype(x.dtype)


def layer_norm(x, g, b):
    xf = x.astype(jnp.float32)
    mu = jnp.mean(xf, axis=-1, keepdims=True)
    var = jnp.mean(jnp.square(xf - mu), axis=-1, keepdims=True)
    y = (xf - mu) * lax.rsqrt(var + LN_EPS)
    return (y * g.astype(jnp.float32) + b.astype(jnp.float32)).astype(x.dtype)


def rope_cos_sin(pos, dim, theta):
    inv = theta ** (-jnp.arange(0, dim, 2, dtype=jnp.float32) / dim)
    ang = pos.astype(jnp.float32)[:, None] * inv[None, :]
    return jnp.cos(ang), jnp.sin(ang)


def rotate_half_rope(x, cos, sin):
    xf = x.astype(jnp.float32)
    x1, x2 = jnp.split(xf, 2, axis=-1)
    out = jnp.concatenate([x1 * cos - x2 * sin, x2 * cos + x1 * sin], axis=-1)
    return out.astype(x.dtype)


def axial_rope(x, row_cs, col_cs):
    half = x.shape[-1] // 2
    xr = rotate_half_rope(x[..., :half], *row_cs)
    xc = rotate_half_rope(x[..., half:], *col_cs)
    return jnp.concatenate([xr, xc], axis=-1)


def partial_rope(x, cs):
    xr = rotate_half_rope(x[..., :PARTIAL_ROPE_DIMS], *cs)
    return jnp.concatenate([xr, x[..., PARTIAL_ROPE_DIMS:]], axis=-1)


def gqa_attention(q, k, v):
    b, hk, g, s, d = q.shape
    nb = s // Q_BLOCK
    qb = jnp.moveaxis(q.reshape(b, hk, g, nb, Q_BLOCK, d), 3, 0)
    scale = d ** -0.5

    def one_block(qblk):
        sc = jnp.einsum('bhgqd,bhkd->bhgqk', qblk, k).astype(jnp.float32) * scale
        p = jax.nn.softmax(sc, axis=-1).astype(v.dtype)
        return jnp.einsum('bhgqk,bhkd->bhgqd', p, v)

    o = lax.map(one_block, qb)
    return jnp.moveaxis(o, 0, 3).reshape(b, hk, g, s, d)


def diff_attention(q, k, v, lam):
    b, h, _, s, d = q.shape
    nb = s // Q_BLOCK
    qb = jnp.moveaxis(q.reshape(b, h, 2, nb, Q_BLOCK, d), 3, 0)
    scale = d ** -0.5

    def one_block(qblk):
        sc = jnp.einsum('bhcqd,bhckd->bhcqk', qblk, k).astype(jnp.float32) * scale
        p = jax.nn.softmax(sc, axis=-1)
        a = p[:, :, 0] - lam * p[:, :, 1]
        return jnp.einsum('bhqk,bhkd->bhqd', a.astype(v.dtype), v)

    o = lax.map(one_block, qb)
    return jnp.moveaxis(o, 0, 2).reshape(b, h, s, v.shape[-1])


def expert_choice_ffn(x, w_router, w_gate, w_up, w_down):
    b, s, d = x.shape
    cap = CAPACITY_FACTOR * s // N_EXPERTS
    logits = jnp.einsum('bsd,de->bse', x, w_router).astype(jnp.float32)
    aff = jax.nn.softmax(logits, axis=-1)
    gate, idx = lax.top_k(jnp.swapaxes(aff, 1, 2), cap)
    xg = jax.vmap(lambda xb, ib: xb[ib])(x, idx)
    hid = jax.nn.silu(jnp.einsum('becd,edf->becf', xg, w_gate)) * jnp.einsum('becd,edf->becf', xg, w_up)
    out = jnp.einsum('becf,efd->becd', hid, w_down) * gate[..., None].astype(x.dtype)
    y = jax.vmap(lambda ib, ob: jnp.zeros((s, d), x.dtype).at[ib.reshape(-1)].add(ob.reshape(-1, d)))(idx, out)
    return y


def setup_inputs(seed: int = 0) -> dict:
    key = jax.random.key(seed)
    ks = jax.random.split(key, 20)
    f32 = jnp.float32
    nrm = lambda k, shape, scale: jax.random.normal(k, shape, f32) * scale
    gain = lambda k, shape: 1.0 + 0.02 * jax.random.normal(k, shape, f32)
    return {
        "x": jax.random.normal(ks[0], (BATCH, SEQ, D_MODEL), f32),
        "w_in": nrm(ks[1], (DEPTH, D_MODEL, IN_COLS), D_MODEL ** -0.5),
        "b_gate": nrm(ks[2], (DEPTH, COL_GATES), 0.02),
        "a_q_norm": gain(ks[3], (DEPTH, HEAD_DIM)),
        "a_k_norm": gain(ks[4], (DEPTH, HEAD_DIM)),
        "b_lambda": nrm(ks[5], (DEPTH, 4, B_QK_DIM), 0.1),
        "b_subln": gain(ks[6], (DEPTH, B_V_DIM)),
        "w_a_proj": nrm(ks[7], (DEPTH, A_WIDTH, D_MODEL), A_WIDTH ** -0.5 * DEEPNORM_BETA),
        "w_b_proj": nrm(ks[8], (DEPTH, B_WIDTH, D_MODEL), B_WIDTH ** -0.5 * DEEPNORM_BETA),
        "w_o": nrm(ks[9], (DEPTH, D_MODEL, D_MODEL), D_MODEL ** -0.5 * DEEPNORM_BETA),
        "ln1_g": gain(ks[10], (DEPTH, D_MODEL)),
        "ln1_b": nrm(ks[11], (DEPTH, D_MODEL), 0.02),
        "w_router": nrm(ks[12], (DEPTH, D_MODEL, N_EXPERTS), D_MODEL ** -0.5),
        "w_gate": nrm(ks[13], (DEPTH, N_EXPERTS, D_MODEL, EXPERT_FF), D_MODEL ** -0.5),
        "w_up": nrm(ks[14], (DEPTH, N_EXPERTS, D_MODEL, EXPERT_FF), D_MODEL ** -0.5),
        "w_down": nrm(ks[15], (DEPTH, N_EXPERTS, EXPERT_FF, D_MODEL), EXPERT_FF ** -0.5 * DEEPNORM_BETA),
        "ln2_g": gain(ks[16], (DEPTH, D_MODEL)),
        "ln2_b": nrm(ks[17], (DEPTH, D_MODEL), 0.02),
    }


def reference(x, w_in, b_gate, a_q_norm, a_k_norm, b_lambda, b_subln, w_a_proj, w_b_proj,
              w_o, ln1_g, ln1_b, w_router, w_gate, w_up, w_down, ln2_g, ln2_b):
    bsz, s, _ = x.shape
    rows = s // GRID_W
    row_idx = jnp.broadcast_to(jnp.arange(rows)[:, None], (rows, GRID_W)).reshape(-1)
    col_idx = jnp.broadcast_to(jnp.arange(GRID_W)[None, :], (rows, GRID_W)).reshape(-1)
    row_cs = rope_cos_sin(row_idx, HEAD_DIM // 2, A_ROPE_THETA)
    col_cs = rope_cos_sin(col_idx, HEAD_DIM // 2, A_ROPE_THETA)
    lin_cs = rope_cos_sin(jnp.arange(s), PARTIAL_ROPE_DIMS, PARTIAL_ROPE_THETA)
    offsets = list(np.cumsum([COL_A_Q, COL_A_K, COL_A_V, COL_B_Q, COL_B_K, COL_B_V]))

    for l in range(DEPTH):
        lam_init = 0.8 - 0.6 * math.exp(-0.3 * l)
        proj = jnp.einsum('bsd,dc->bsc', x, w_in[l])
        qa, ka, va, qb, kb, vb, gates = jnp.split(proj, [int(o) for o in offsets], axis=-1)

        qa = qa.reshape(bsz, s, A_KV_HEADS, A_GROUP, HEAD_DIM).transpose(0, 2, 3, 1, 4)
        ka = ka.reshape(bsz, s, A_KV_HEADS, HEAD_DIM).transpose(0, 2, 1, 3)
        va = va.reshape(bsz, s, A_KV_HEADS, HEAD_DIM).transpose(0, 2, 1, 3)
        qa = axial_rope(rms_norm(qa, a_q_norm[l]), row_cs, col_cs)
        ka = axial_rope(rms_norm(ka, a_k_norm[l]), row_cs, col_cs)
        oa = gqa_attention(qa, ka, va)
        oa = oa.transpose(0, 3, 1, 2, 4).reshape(bsz, s, A_WIDTH)
        ya = jnp.einsum('bsw,wd->bsd', oa, w_a_proj[l])

        qb = partial_rope(qb.reshape(bsz, s, B_HEADS, 2, B_QK_DIM).transpose(0, 2, 3, 1, 4), lin_cs)
        kb = partial_rope(kb.reshape(bsz, s, B_HEADS, 2, B_QK_DIM).transpose(0, 2, 3, 1, 4), lin_cs)
        vb = vb.reshape(bsz, s, B_HEADS, B_V_DIM).transpose(0, 2, 1, 3)
        lp = b_lambda[l].astype(jnp.float32)
        lam = jnp.exp(jnp.sum(lp[0] * lp[1])) - jnp.exp(jnp.sum(lp[2] * lp[3])) + lam_init
        ob = diff_attention(qb, kb, vb, lam)
        ob = rms_norm(ob, b_subln[l]) * (1.0 - lam_init)
        ob = ob.transpose(0, 2, 1, 3).reshape(bsz, s, B_WIDTH)
        yb = jnp.einsum('bsw,wd->bsd', ob, w_b_proj[l])

        g = jax.nn.sigmoid((gates + b_gate[l]).reshape(bsz, s, N_BRANCHES, D_MODEL))
        merged = g[:, :, 0] * ya + g[:, :, 1] * yb
        mix = jnp.einsum('bsd,de->bse', merged, w_o[l])
        x = layer_norm(DEEPNORM_ALPHA * x + mix, ln1_g[l], ln1_b[l])

        ffn = expert_choice_ffn(x, w_router[l], w_gate[l], w_up[l], w_down[l])
        x = layer_norm(DEEPNORM_ALPHA * x + ffn, ln2_g[l], ln2_b[l])
    return x
```

```python
import math
from contextlib import ExitStack

import numpy as np
import ml_dtypes

import concourse.bass as bass
import concourse.mybir as mybir
from concourse.bass_utils import run_bass_kernel_spmd

F32 = mybir.dt.float32
BF16 = mybir.dt.bfloat16
U32 = mybir.dt.uint32
AF = mybir.ActivationFunctionType
ALU = mybir.AluOpType
AX = mybir.AxisListType

S = 2048
D = 2048
NT = 16
KD = 16
NCORE = 8
NE = 16
CAP = 256
FF = 4096
IN_COLS = 8704
ALPHA = 2.0 ** 0.25
LAM_INIT = 0.2
RMS_EPS = 1e-6
LN_EPS = 1e-5
SCALE = 128.0 ** -0.5


class Sched:
    ENG = ("pe", "act", "dve", "pool", "sp")

    def __init__(self, nc, es):
        self.nc = nc
        self.es = es
        self.prog = {e: [] for e in self.ENG}
        self.semh = {}
        self.cnt = {}
        for e in self.ENG:
            self.semh[e] = es.enter_context(nc.semaphore("sem_" + e))
            self.cnt[e] = 0
        self.waited = {e: {} for e in self.ENG}
        self.lastw = {}
        self.readers = {}

    def _slot(self, name):
        if name not in self.semh:
            self.semh[name] = self.es.enter_context(self.nc.semaphore("d_" + name))
            self.cnt[name] = 0
        return name

    def op(self, eng, fn, reads=(), writes=(), dma=None):
        deps = {}

        def add(tok):
            if tok is None:
                return
            name, val = tok
            if name == "pe" and eng == "pe":
                return
            if deps.get(name, 0) < val:
                deps[name] = val

        for k in reads:
            add(self.lastw.get(k))
        for k in writes:
            add(self.lastw.get(k))
            for t in self.readers.get(k, {}).items():
                add(t)
        for name, val in deps.items():
            if self.waited[eng].get(name, 0) >= val:
                continue
            self.waited[eng][name] = val
            sem = self.semh[name]
            self.prog[eng].append(lambda e, sem=sem, val=val: e.wait_ge(sem, val))
        if dma is not None:
            name = self._slot(dma)
            self.cnt[name] += 16
            inc = 16
        else:
            name = eng
            self.cnt[name] += 1
            inc = 1
        tok = (name, self.cnt[name])
        sem = self.semh[name]
        self.prog[eng].append(lambda e, fn=fn, sem=sem, inc=inc: fn(e).then_inc(sem, inc))
        for k in reads:
            self.readers.setdefault(k, {})[name] = tok[1]
        for k in writes:
            self.lastw[k] = tok
            self.readers[k] = {}
        return tok

    def barrier(self):
        for eng in self.ENG:
            for name, sem in self.semh.items():
                val = self.cnt[name]
                if val == 0 or self.waited[eng].get(name, 0) >= val:
                    continue
                self.waited[eng][name] = val
                self.prog[eng].append(lambda e, sem=sem, val=val: e.wait_ge(sem, val))

    def final_wait(self, eng, names):
        for name in names:
            val = self.cnt[name]
            sem = self.semh[name]
            self.prog[eng].append(lambda e, sem=sem, val=val: e.wait_ge(sem, val))

    def emit(self):
        nc = self.nc
        with nc.Block() as block:
            @block.tensor
            def _(e):
                for f in self.prog["pe"]:
                    f(e)

            @block.scalar
            def _(e):
                for f in self.prog["act"]:
                    f(e)

            @block.vector
            def _(e):
                for f in self.prog["dve"]:
                    f(e)

            @block.gpsimd
            def _(e):
                for f in self.prog["pool"]:
                    f(e)

            @block.sync
            def _(e):
                for f in self.prog["sp"]:
                    f(e)


def build_p1(debug=False):
    nc = bass.Bass("TRN2", target_bir_lowering=False)
    es = ExitStack()
    with es:
        _build(nc, es, debug)
    return nc


def _build(nc, es, debug):
    def din(name, shape, dt=F32):
        return nc.dram_tensor(name, list(shape), dt, kind="ExternalInput").ap()

    def dint(name, shape, dt):
        return nc.dram_tensor(name, list(shape), dt, kind="Internal").ap()

    x = din("x", [S, D])
    w_in = din("w_in", [D, IN_COLS])
    bgT = din("bgT", [128, 32])
    gq_d = din("gq", [128, 1])
    gk_d = din("gk", [128, 1])
    lamb_d = din("lamb", [128, 512])
    subln_d = din("subln", [128, 2])
    w_a = din("w_a", [1024, D])
    w_b = din("w_b", [1024, D])
    w_o = din("w_o", [D, D])
    ln1g_d = din("ln1g", [128, D])
    ln1b_d = din("ln1b", [128, D])
    w_r = din("w_r", [D, NE])
    ropeA_c = din("ropeA_c", [128, S])
    ropeA_s = din("ropeA_s", [128, S])
    ropeB_c = din("ropeB_c", [128, S])
    ropeB_s = din("ropeB_s", [128, S])
    permA_d = din("permA", [128, 128])
    permB_d = din("permB", [128, 128])
    identf_d = din("identf", [128, 128])
    identb_d = din("identb", [128, 128], BF16)
    Y = nc.dram_tensor("y_out", [S, D], F32, kind="ExternalOutput").ap()
    xg_out = nc.dram_tensor("xg_out", [NE * CAP, D], BF16, kind="ExternalOutput").ap()
    idx_out = nc.dram_tensor("idx_out", [NE, CAP], U32, kind="ExternalOutput").ap()
    gate_out = nc.dram_tensor("gate_out", [NE, CAP], F32, kind="ExternalOutput").ap()
    if debug:
        dbg = nc.dram_tensor("dbg", [S, D], F32, kind="ExternalOutput").ap()
        dbg_ot = nc.dram_tensor("dbg_ot", [16 * 128, S], BF16, kind="ExternalOutput").ap()
        dbg_g = nc.dram_tensor("dbg_g", [32 * 128, S], F32, kind="ExternalOutput").ap()
        dbg_z = nc.dram_tensor("dbg_z", [S, D], F32, kind="ExternalOutput").ap()

    OT = dint("OT", [16 * 128, S], BF16)
    GATES = dint("GATES", [32 * 128, S], F32)
    X1B = dint("X1B", [S, D], BF16)
    LOGT = dint("LOGT", [NE, S], F32)

    sc = Sched(nc, es)
    scope = [es]
    sb = lambda name, shape, dt=F32: scope[0].enter_context(nc.sbuf_tensor("s_" + name, list(shape), dt))
    ps = [es.enter_context(nc.psum_tensor("ps%d" % i, [128, 512], F32)) for i in range(8)]
    PSK = [("ps", i) for i in range(8)]

    identf = sb("identf", [128, 128])
    identb = sb("identb", [128, 128], BF16)
    permA = sb("permA", [128, 128])
    permB = sb("permB", [128, 128])
    onesf = sb("onesf", [128, 128])
    onesb = sb("onesb", [128, 128], BF16)
    bg = sb("bg", [128, 32])
    gq = sb("gqs", [128, 1])
    gk = sb("gks", [128, 1])
    lamb = sb("lambs", [128, 512])
    subg = sb("subg", [128, 2])
    neglam = sb("neglam", [128, 1])
    smalls = sb("smalls", [128, 8])
    wr_sb = sb("wr_sb", [128, KD, NE])

    def ld(dst, src, key, q="sp"):
        sc.op(q, lambda e: e.dma_start(out=dst, in_=src), writes=[key], dma="c_" + key)

    ld(identf[:], identf_d, "identf")
    ld(identb[:], identb_d, "identb")
    ld(permA[:], permA_d, "permA")
    ld(permB[:], permB_d, "permB")
    ld(bg[:], bgT, "bg")
    ld(gq[:], gq_d, "gq")
    ld(gk[:], gk_d, "gk")
    ld(lamb[:], lamb_d, "lamb")
    ld(subg[:], subln_d, "subg")
    ld(wr_sb[:], w_r.rearrange("(k p) e -> p k e", p=128), "wr")
    sc.op("pool", lambda e: e.memset(onesf[:], 1.0), writes=["onesf"])
    sc.op("pool", lambda e: e.memset(onesb[:], 1.0), writes=["onesb"])
    lt = sb("lam_t", [128, 256])
    sc.op("dve", lambda e: e.tensor_tensor(out=lt[:, 0:128], in0=lamb[:, 0:128], in1=lamb[:, 128:256], op=ALU.mult),
          reads=["lamb"], writes=["lt"])
    sc.op("dve", lambda e: e.tensor_tensor(out=lt[:, 128:256], in0=lamb[:, 256:384], in1=lamb[:, 384:512], op=ALU.mult),
          reads=["lamb"], writes=["lt"])
    sc.op("dve", lambda e: e.reduce_sum(out=smalls[:, 0:1], in_=lt[:, 0:128], axis=AX.X), reads=["lt"], writes=["sm0"])
    sc.op("dve", lambda e: e.reduce_sum(out=smalls[:, 1:2], in_=lt[:, 128:256], axis=AX.X), reads=["lt"], writes=["sm1"])
    sc.op("act", lambda e: e.activation(out=smalls[:, 2:4], in_=smalls[:, 0:2], func=AF.Exp), reads=["sm0", "sm1"], writes=["sm2"])
    sc.op("dve", lambda e: e.tensor_tensor(out=smalls[:, 4:5], in0=smalls[:, 3:4], in1=smalls[:, 2:3], op=ALU.subtract),
          reads=["sm2"], writes=["sm4"])
    sc.op("dve", lambda e: e.tensor_scalar(out=neglam[:], in0=smalls[:, 4:5], scalar1=-LAM_INIT, scalar2=None, op0=ALU.add),
          reads=["sm4"], writes=["neglam"])
    sc.op("dve", lambda e: e.tensor_scalar(out=subg[:], in0=subg[:], scalar1=1.0 - LAM_INIT, scalar2=None, op0=ALU.mult),
          reads=["subg"], writes=["subg"])

    xT = sb("xT", [128, KD, S], BF16)
    NW = 3
    wbuf = [sb("wbuf%d" % i, [128, 16, 512], BF16) for i in range(NW)]
    xs = [sb("xs%d" % i, [128, D]) for i in range(2)]
    NTMP = 8
    tmp = [sb("tmp%d" % i, [128, 512]) for i in range(NTMP)]
    es_p1 = ExitStack()
    scope[0] = es_p1
    Qb = sb("Qb", [128, 4, S], BF16)
    Kb = sb("Kb", [128, 2, S], BF16)
    Vb = sb("Vb", [128, NT, 256], BF16)
    NPB = 4
    pT = [sb("pT%d" % i, [128, 512], BF16) for i in range(NPB)]
    ost = [sb("ost%d" % i, [128, 512], BF16) for i in range(2)]
    o0 = [sb("o0_%d" % i, [128, 512]) for i in range(2)]
    o1 = [sb("o1_%d" % i, [128, 512]) for i in range(2)]
    rc = [sb("rc%d" % i, [128, 512]) for i in range(2)]
    rs_ = [sb("rs%d" % i, [128, 512]) for i in range(2)]

    state = {"w": 0, "tmp": 0, "pT": 0, "ost": 0}

    def load_w(src3, nk, ncols, pieces=None):
        i = state["w"] % NW
        state["w"] += 1
        key = ("w", i)
        if pieces is None:
            pieces = [(0, src3)]
        for (k0, srcp) in pieces:
            nkp = srcp.shape[1]
            sc.op("pool", lambda e, srcp=srcp, k0=k0, nkp=nkp: e.dma_start(out=wbuf[i][:, k0:k0 + nkp, 0:ncols], in_=srcp),
                  writes=[key], dma="w%d" % i)
        return wbuf[i], key

    def get_tmp():
        i = state["tmp"] % NTMP
        state["tmp"] += 1
        return tmp[i], ("tmp", i)

    def wslab(w2d, c0, ncols):
        return w2d[:, c0:c0 + ncols].rearrange("(k p) c -> p k c", p=128)

    for t in range(NT):
        xst = xs[t % 2]
        xk = ("xs", t % 2)
        sc.op("sp", lambda e, xst=xst, t=t: e.dma_start(out=xst[:], in_=x[t * 128:(t + 1) * 128, :]),
              writes=[xk], dma="xs%d" % (t % 2))
        for j in range(4):
            bank = (t * 4 + j) % 4

            def tr(e, xst=xst, j=j, bank=bank):
                for i in range(4):
                    ins = e.transpose(out=ps[bank][:, i * 128:(i + 1) * 128],
                                      in_=xst[:, (4 * j + i) * 128:(4 * j + i + 1) * 128], identity=identf[:])
                return ins
            sc.op("pe", tr, reads=[xk, "identf"], writes=[PSK[bank]])
            eng = "act" if j % 2 == 0 else "dve"
            dst = xT[:, 4 * j:4 * j + 4, t * 128:(t + 1) * 128]
            srcv = ps[bank][:].rearrange("p (a b) -> p a b", a=4)
            if eng == "act":
                sc.op("act", lambda e, dst=dst, srcv=srcv: e.activation(out=dst, in_=srcv, func=AF.Copy),
                      reads=[PSK[bank]], writes=[("xT", t)])
            else:
                sc.op("dve", lambda e, dst=dst, srcv=srcv: e.tensor_copy(out=dst, in_=srcv),
                      reads=[PSK[bank]], writes=[("xT", t)])
    XT_ALL = [("xT", t) for t in range(NT)]

    def inproj_fm(wb, wkey, coff, blk, bank):
        def f(e):
            for k in range(KD):
                ins = e.matmul(ps[bank][:], lhsT=wb[:, k, coff:coff + 128], rhs=xT[:, k, blk * 512:(blk + 1) * 512],
                               start=(k == 0), stop=(k == KD - 1))
            return ins
        sc.op("pe", f, reads=[wkey] + [("xT", t) for t in range(blk * 4, blk * 4 + 4)], writes=[PSK[bank]])

    def load_rope(blk, which):
        i = blk % 2
        c_d, s_d = (ropeA_c, ropeA_s) if which == "A" else (ropeB_c, ropeB_s)
        sc.op("sp", lambda e: e.dma_start(out=rc[i][:], in_=c_d[:, blk * 512:(blk + 1) * 512]), writes=[("rc", i)], dma="rc%d" % i)
        sc.op("sp", lambda e: e.dma_start(out=rs_[i][:], in_=s_d[:, blk * 512:(blk + 1) * 512]), writes=[("rs", i)], dma="rs%d" % i)
        return i

    def rope_chunk(bank, bank2, bank3, ri, dst, dkey, gain, perm, permkey, rms):
        qg, qgk = get_tmp()
        if gain is not None:
            sc.op("act", lambda e: e.activation(out=qg[:], in_=ps[bank][:], func=AF.Copy, scale=gain[:]),
                  reads=[PSK[bank], "gq", "gk"], writes=[qgk])
        else:
            sc.op("act", lambda e: e.activation(out=qg[:], in_=ps[bank][:], func=AF.Copy),
                  reads=[PSK[bank]], writes=[qgk])
        if rms:
            sq, sqk = get_tmp()
            sc.op("act", lambda e: e.activation(out=sq[:], in_=ps[bank][:], func=AF.Square),
                  reads=[PSK[bank]], writes=[sqk])
            sc.op("pe", lambda e: e.matmul(ps[bank2][:], lhsT=onesf[:], rhs=sq[:], start=True, stop=True),
                  reads=[sqk, "onesf"], writes=[PSK[bank2]])
            rstd, rk = get_tmp()
            sc.op("act", lambda e: e.activation(out=rstd[:], in_=ps[bank2][:], func=AF.Ln, scale=1.0 / 128.0, bias=smalls[:, 5:6]),
                  reads=[PSK[bank2], "eps_rms"], writes=[rk])
            sc.op("act", lambda e: e.activation(out=rstd[:], in_=rstd[:], func=AF.Exp, scale=-0.5),
                  reads=[rk], writes=[rk])
        sc.op("pe", lambda e: e.matmul(ps[bank3][:], lhsT=perm[:], rhs=qg[:], start=True, stop=True),
              reads=[qgk, permkey], writes=[PSK[bank3]])
        t1, t1k = get_tmp()
        sc.op("pool", lambda e: e.tensor_tensor(out=t1[:], in0=qg[:], in1=rc[ri][:], op=ALU.mult),
              reads=[qgk, ("rc", ri)], writes=[t1k])
        t2, t2k = get_tmp()
        sc.op("dve", lambda e: e.tensor_tensor(out=t2[:], in0=ps[bank3][:], in1=rs_[ri][:], op=ALU.mult),
              reads=[PSK[bank3], ("rs", ri)], writes=[t2k])
        if rms:
            sc.op("dve", lambda e: e.tensor_tensor(out=t1[:], in0=t1[:], in1=t2[:], op=ALU.add),
                  reads=[t1k, t2k], writes=[t1k])
            sc.op("dve", lambda e: e.tensor_tensor(out=dst, in0=t1[:], in1=rstd[:], op=ALU.mult),
                  reads=[t1k, rk], writes=[dkey])
        else:
            sc.op("dve", lambda e: e.tensor_tensor(out=dst, in0=t1[:], in1=t2[:], op=ALU.add),
                  reads=[t1k, t2k], writes=[dkey])

    sc.op("pool", lambda e: e.memset(smalls[:, 5:6], RMS_EPS), writes=["eps_rms"])
    sc.op("pool", lambda e: e.memset(smalls[:, 6:7], LN_EPS), writes=["eps_ln"])

    def inproj_v(wb, wkey, dv):
        per = 512 // dv
        for t0 in range(0, NT, per):
            bank = 4 + (t0 // per) % 2

            def f(e, t0=t0, bank=bank):
                for i in range(per):
                    t = t0 + i
                    for k in range(KD):
                        ins = e.matmul(ps[bank][:, i * dv:(i + 1) * dv], lhsT=xT[:, k, t * 128:(t + 1) * 128],
                                       rhs=wb[:, k, 0:dv], start=(k == 0), stop=(k == KD - 1))
                return ins
            sc.op("pe", f, reads=[wkey] + XT_ALL[t0:t0 + per], writes=[PSK[bank]])
            srcv = ps[bank][:].rearrange("p (a b) -> p a b", a=per)
            sc.op("act", lambda e, t0=t0, srcv=srcv: e.activation(out=Vb[:, t0:t0 + per, 0:dv], in_=srcv, func=AF.Copy),
                  reads=[PSK[bank]], writes=["V"])

    unit_no = [0]

    def attention_unit(qi, ki, blk, dv, epilogue):
        u = unit_no[0] % 2
        unit_no[0] += 1
        nh = dv // 128
        obank = [2 + 2 * u, 3 + 2 * u]
        lbank = 6 + u
        qap = Qb[:, qi, blk * 512:(blk + 1) * 512]
        pk = [None] * NT

        def qk(kt):
            sb_ = kt % 2
            sc.op("pe", lambda e: e.matmul(ps[sb_][:], lhsT=Kb[:, ki, kt * 128:(kt + 1) * 128], rhs=qap, start=True, stop=True),
                  reads=[("Q", qi), ("K", ki)], writes=[PSK[sb_]])
            i = state["pT"] % NPB
            state["pT"] += 1
            pk[kt] = i
            sc.op("act", lambda e: e.activation(out=pT[i][:], in_=ps[sb_][:], func=AF.Exp, scale=SCALE),
                  reads=[PSK[sb_]], writes=[("pT", i)])

        def pv(kt):
            i = pk[kt]

            def f(e):
                for h in range(nh):
                    e.matmul(ps[obank[h]][:], lhsT=Vb[:, kt, h * 128:(h + 1) * 128], rhs=pT[i][:],
                             start=(kt == 0), stop=(kt == NT - 1))
                return e.matmul(ps[lbank][:], lhsT=onesb[:], rhs=pT[i][:], start=(kt == 0), stop=(kt == NT - 1))
            sc.op("pe", f, reads=[("pT", i), "V", "onesb"], writes=[PSK[obank[h]] for h in range(nh)] + [PSK[lbank]])

        qk(0)
        qk(1)
        for kt in range(NT):
            pv(kt)
            if kt + 2 < NT:
                qk(kt + 2)
        rl, rlk = get_tmp()
        sc.op("dve", lambda e: e.reciprocal(out=rl[:], in_=ps[lbank][:]), reads=[PSK[lbank]], writes=[rlk])
        epilogue(obank, rl, rlk)

    def store_oT(src_tile, skey, chunk, blk):
        sc.op("sp", lambda e: e.dma_start(out=OT[chunk * 128:(chunk + 1) * 128, blk * 512:(blk + 1) * 512], in_=src_tile[:]),
              reads=[skey], dma="ost_%s" % str(skey))

    for hk in range(2):
        wq, wqk = load_w(wslab(w_in, hk * 512, 512), KD, 512)
        wk_, wkk = load_w(wslab(w_in, 1024 + hk * 128, 128), KD, 128)
        wv_, wvk = load_w(wslab(w_in, 1280 + hk * 128, 128), KD, 128)
        for blk in range(4):
            ri = load_rope(blk, "A")
            for g in range(4):
                inproj_fm(wq, wqk, g * 128, blk, 0)
                rope_chunk(0, 1, 2, ri, Qb[:, g, blk * 512:(blk + 1) * 512], ("Q", g), gq, permA, "permA", True)
            inproj_fm(wk_, wkk, 0, blk, 0)
            rope_chunk(0, 1, 2, ri, Kb[:, 0, blk * 512:(blk + 1) * 512], ("K", 0), gk, permA, "permA", True)
        inproj_v(wv_, wvk, 128)
        for g in range(4):
            for blk in range(4):
                def epiA(obank, rl, rlk, g=g, blk=blk):
                    i = state["ost"] % 2
                    state["ost"] += 1
                    sc.op("dve", lambda e: e.tensor_tensor(out=ost[i][:], in0=ps[obank[0]][:], in1=rl[:], op=ALU.mult),
                          reads=[PSK[obank[0]], rlk], writes=[("ost", i)])
                    store_oT(ost[i], ("ost", i), hk * 4 + g, blk)
                attention_unit(g, 0, blk, 128, epiA)

    for h in range(4):
        wq, wqk = load_w(wslab(w_in, 1536 + h * 256, 256), KD, 256)
        wk_, wkk = load_w(wslab(w_in, 2560 + h * 256, 256), KD, 256)
        wv_, wvk = load_w(wslab(w_in, 3584 + h * 256, 256), KD, 256)
        for blk in range(4):
            ri = load_rope(blk, "B")
            for c in range(2):
                inproj_fm(wq, wqk, c * 128, blk, 0)
                rope_chunk(0, 1, 2, ri, Qb[:, c, blk * 512:(blk + 1) * 512], ("Q", c), None, permB, "permB", False)
                inproj_fm(wk_, wkk, c * 128, blk, 0)
                rope_chunk(0, 1, 2, ri, Kb[:, c, blk * 512:(blk + 1) * 512], ("K", c), None, permB, "permB", False)
        inproj_v(wv_, wvk, 256)
        for blk in range(4):
            def epiB0(obank, rl, rlk):
                for hf in range(2):
                    sc.op("dve", lambda e, hf=hf: e.tensor_tensor(out=o0[hf][:], in0=ps[obank[hf]][:], in1=rl[:], op=ALU.mult),
                          reads=[PSK[obank[hf]], rlk], writes=[("o0", hf)])

            def epiB1(obank, rl, rlk, h=h, blk=blk):
                sqs = []
                for hf in range(2):
                    sc.op("dve", lambda e, hf=hf: e.tensor_tensor(out=o1[hf][:], in0=ps[obank[hf]][:], in1=rl[:], op=ALU.mult),
                          reads=[PSK[obank[hf]], rlk], writes=[("o1", hf)])
                    sc.op("dve", lambda e, hf=hf: e.scalar_tensor_tensor(out=o1[hf][:], in0=o1[hf][:], scalar=neglam[:], in1=o0[hf][:],
                                                                      op0=ALU.mult, op1=ALU.add),
                          reads=[("o1", hf), ("o0", hf), "neglam"], writes=[("o1", hf)])
                    sq, sqk = get_tmp()
                    sc.op("act", lambda e, hf=hf, sq=sq: e.activation(out=sq[:], in_=o1[hf][:], func=AF.Square),
                          reads=[("o1", hf)], writes=[sqk])
                    sqs.append((sq, sqk))
                lb = obank[0]

                def f(e):
                    e.matmul(ps[lb][:], lhsT=onesf[:], rhs=sqs[0][0][:], start=True, stop=False)
                    return e.matmul(ps[lb][:], lhsT=onesf[:], rhs=sqs[1][0][:], start=False, stop=True)
                sc.op("pe", f, reads=[sqs[0][1], sqs[1][1], "onesf"], writes=[PSK[lb]])
                rstd, rk = get_tmp()
                sc.op("act", lambda e: e.activation(out=rstd[:], in_=ps[lb][:], func=AF.Ln, scale=1.0 / 256.0, bias=smalls[:, 5:6]),
                      reads=[PSK[lb], "eps_rms"], writes=[rk])
                sc.op("act", lambda e: e.activation(out=rstd[:], in_=rstd[:], func=AF.Exp, scale=-0.5), reads=[rk], writes=[rk])
                for hf in range(2):
                    i = state["ost"] % 2
                    state["ost"] += 1
                    sc.op("dve", lambda e, hf=hf, i=i: e.scalar_tensor_tensor(out=ost[i][:], in0=o1[hf][:], scalar=subg[:, hf:hf + 1], in1=rstd[:],
                                                                           op0=ALU.mult, op1=ALU.mult),
                          reads=[("o1", hf), "subg", rk], writes=[("ost", i)])
                    store_oT(ost[i], ("ost", i), 8 + h * 2 + hf, blk)
            attention_unit(0, 0, blk, 256, epiB0)
            attention_unit(1, 1, blk, 256, epiB1)

    for cg in range(8):
        wgt, wgk = load_w(wslab(w_in, 4608 + cg * 512, 512), KD, 512)
        for jj in range(4):
            j = cg * 4 + jj
            for blk in range(4):
                bank = (jj * 4 + blk) % 4
                inproj_fm(wgt, wgk, jj * 128, blk, bank)
                tt_, tk = get_tmp()
                sc.op("act", lambda e, tt_=tt_, bank=bank, j=j: e.activation(out=tt_[:], in_=ps[bank][:], func=AF.Sigmoid, bias=bg[:, j:j + 1]),
                      reads=[PSK[bank], "bg"], writes=[tk])
                sc.op("sp", lambda e, tt_=tt_, j=j, blk=blk: e.dma_start(out=GATES[j * 128:(j + 1) * 128, blk * 512:(blk + 1) * 512], in_=tt_[:]),
                      reads=[tk], writes=[("GATES", j, blk)], dma="gst_%s" % str(tk))

    sc.barrier()
    es_p1.close()
    if debug:
        sc.op("sp", lambda e: e.dma_start(out=dbg_ot, in_=OT), dma="dbg_st")
        sc.op("sp", lambda e: e.dma_start(out=dbg_g, in_=GATES), dma="dbg_st")
    es_p2 = es.enter_context(ExitStack())
    scope[0] = es_p2
    oTg = [xT[:, 0:16, 0:512], xT[:, 0:16, 512:1024]]
    mrg = xT[:, 0:16, 1024:1536]
    zt = [sb("z%d" % i, [128, D]) for i in range(4)]
    x1f = xs[0]
    x1b = sb("x1b", [128, D], BF16)
    ya_t = xs[1]
    lng = sb("lng", [128, D])
    lnb = sb("lnb", [128, D])
    stats = sb("stats", [128, 4, 6])
    mv = sb("mv", [128, 8])
    ld(lng[:], ln1g_d, "lng")
    ld(lnb[:], ln1b_d, "lnb")

    def layer_norm_tile(zt_ap, zkey, dst, dkey):
        for c in range(4):
            sc.op("dve", lambda e, c=c: e.bn_stats(out=stats[:, c, :], in_=zt_ap[:, c * 512:(c + 1) * 512]),
                  reads=[zkey], writes=["stats"])
        sc.op("dve", lambda e: e.bn_aggr(out=mv[:, 0:2], in_=stats[:].rearrange("p a b -> p (a b)")), reads=["stats"], writes=["mv"])
        sc.op("act", lambda e: e.activation(out=mv[:, 2:3], in_=mv[:, 1:2], func=AF.Ln, bias=smalls[:, 6:7]),
              reads=["mv", "eps_ln"], writes=["mv2"])
        sc.op("act", lambda e: e.activation(out=mv[:, 2:3], in_=mv[:, 2:3], func=AF.Exp, scale=-0.5), reads=["mv2"], writes=["mv2"])
        sc.op("dve", lambda e: e.scalar_tensor_tensor(out=mv[:, 3:4], in0=mv[:, 0:1], scalar=-1.0, in1=mv[:, 2:3], op0=ALU.mult, op1=ALU.mult),
              reads=["mv", "mv2"], writes=["mv3"])
        sc.op("act", lambda e: e.activation(out=dst, in_=zt_ap, func=AF.Identity, scale=mv[:, 2:3], bias=mv[:, 3:4]),
              reads=[zkey, "mv2", "mv3"], writes=[dkey])
        sc.op("pool", lambda e: e.tensor_tensor(out=dst, in0=dst, in1=lng[:], op=ALU.mult), reads=[dkey, "lng"], writes=[dkey])
        sc.op("dve", lambda e: e.tensor_tensor(out=dst, in0=dst, in1=lnb[:], op=ALU.add), reads=[dkey, "lnb"], writes=[dkey])

    for g in range(4):
        og = oTg[g % 2]
        ogk = ("oTg", g % 2)
        sc.op("sp", lambda e, og=og, g=g: e.dma_start(out=og, in_=OT[:, g * 512:(g + 1) * 512].rearrange("(k p) s -> p k s", p=128)),
              writes=[ogk], dma="oTg%d" % (g % 2))
        for jj in range(8):
            wab, wabk = load_w(None, 0, 256, pieces=[(0, w_a[:, jj * 256:(jj + 1) * 256].rearrange("(k p) c -> p k c", p=128)),
                                                     (8, w_b[:, jj * 256:(jj + 1) * 256].rearrange("(k p) c -> p k c", p=128))])
            for j2 in range(2):
                j = jj * 2 + j2

                def fa(e, j2=j2, og=og, wab=wab):
                    for k in range(8):
                        ins = e.matmul(ps[0][:], lhsT=wab[:, k, j2 * 128:(j2 + 1) * 128], rhs=og[:, k, :], start=(k == 0), stop=(k == 7))
                    return ins

                def fb(e, j2=j2, og=og, wab=wab):
                    for k in range(8):
                        ins = e.matmul(ps[1][:], lhsT=wab[:, 8 + k, j2 * 128:(j2 + 1) * 128], rhs=og[:, 8 + k, :], start=(k == 0), stop=(k == 7))
                    return ins
                sc.op("pe", fa, reads=[wabk, ogk], writes=[PSK[0]])
                sc.op("pe", fb, reads=[wabk, ogk], writes=[PSK[1]])
                g0, g0k = get_tmp()
                g1, g1k = get_tmp()
                sc.op("sp", lambda e, g0=g0, j=j, g=g: e.dma_start(out=g0[:], in_=GATES[j * 128:(j + 1) * 128, g * 512:(g + 1) * 512]),
                      reads=[("GATES", j, g)], writes=[g0k], dma="gld_%s" % str(g0k))
                sc.op("sp", lambda e, g1=g1, j=j, g=g: e.dma_start(out=g1[:], in_=GATES[(16 + j) * 128:(17 + j) * 128, g * 512:(g + 1) * 512]),
                      reads=[("GATES", 16 + j, g)], writes=[g1k], dma="gld_%s" % str(g1k))
                sc.op("dve", lambda e, g0=g0: e.tensor_tensor(out=g0[:], in0=ps[0][:], in1=g0[:], op=ALU.mult), reads=[PSK[0], g0k], writes=[g0k])
                sc.op("dve", lambda e, g1=g1: e.tensor_tensor(out=g1[:], in0=ps[1][:], in1=g1[:], op=ALU.mult), reads=[PSK[1], g1k], writes=[g1k])
                sc.op("pool", lambda e, g0=g0, g1=g1, j=j: e.tensor_tensor(out=mrg[:, j, :], in0=g0[:], in1=g1[:], op=ALU.add),
                      reads=[g0k, g1k], writes=[("mrg", j)])
        MRG = [("mrg", j) for j in range(16)]
        for eb in range(4):
            wob, wobk = load_w(wslab(w_o, eb * 512, 512), KD, 512)
            for tt in range(4):
                t = g * 4 + tt
                bank = 2 + (eb * 4 + tt) % 4

                def fo(e, tt=tt, bank=bank, wob=wob):
                    for k in range(KD):
                        ins = e.matmul(ps[bank][:], lhsT=mrg[:, k, tt * 128:(tt + 1) * 128], rhs=wob[:, k, :], start=(k == 0), stop=(k == KD - 1))
                    return ins
                sc.op("pe", fo, reads=[wobk] + MRG, writes=[PSK[bank]])
                xr, xrk = get_tmp()
                sc.op("sp", lambda e, xr=xr, t=t, eb=eb: e.dma_start(out=xr[:], in_=x[t * 128:(t + 1) * 128, eb * 512:(eb + 1) * 512]),
                      writes=[xrk], dma="xr_%s" % str(xrk))
                sc.op("dve", lambda e, xr=xr, tt=tt, eb=eb, bank=bank: e.scalar_tensor_tensor(
                    out=zt[tt][:, eb * 512:(eb + 1) * 512], in0=xr[:], scalar=ALPHA, in1=ps[bank][:], op0=ALU.mult, op1=ALU.add),
                    reads=[xrk, PSK[bank]], writes=[("z", tt)])
        for tt in range(4):
            t = g * 4 + tt
            if debug:
                sc.op("sp", lambda e, tt=tt, t=t: e.dma_start(out=dbg_z[t * 128:(t + 1) * 128, :], in_=zt[tt][:]),
                      reads=[("z", tt)], dma="dbg_st")
            layer_norm_tile(zt[tt][:], ("z", tt), x1f[:], "x1f")
            sc.op("act", lambda e: e.activation(out=x1b[:], in_=x1f[:], func=AF.Copy), reads=["x1f"], writes=["x1b"])
            sc.op("sp", lambda e, t=t: e.dma_start(out=X1B[t * 128:(t + 1) * 128, :], in_=x1b[:]), reads=["x1b"], writes=[("X1B", t)], dma="x1b_st")
            sc.op("pool", lambda e: e.tensor_scalar(out=ya_t[:], in0=x1f[:], scalar1=ALPHA, scalar2=None, op0=ALU.mult), reads=["x1f"], writes=["ya_t"])
            sc.op("sp", lambda e, t=t: e.dma_start(out=Y[t * 128:(t + 1) * 128, :], in_=ya_t[:]), reads=["ya_t"], writes=[("Y", t)], dma="ya_st")
            xTt = []
            for j in range(4):
                bank = j % 2
                xt_, xtk = get_tmp()
                xTt.append((xt_, xtk))

                def trr(e, j=j, bank=bank):
                    for i in range(4):
                        ins = e.transpose(out=ps[bank][:, i * 128:(i + 1) * 128], in_=x1f[:, (4 * j + i) * 128:(4 * j + i + 1) * 128], identity=identf[:])
                    return ins
                sc.op("pe", trr, reads=["x1f", "identf"], writes=[PSK[bank]])
                sc.op("act", lambda e, xt_=xt_, bank=bank: e.activation(out=xt_[:], in_=ps[bank][:], func=AF.Copy), reads=[PSK[bank]], writes=[xtk])

            def fr(e, xTt=xTt):
                for k in range(KD):
                    ins = e.matmul(ps[6][0:NE, 0:128], lhsT=wr_sb[:, k, :], rhs=xTt[k // 4][0][:, (k % 4) * 128:(k % 4 + 1) * 128],
                                   start=(k == 0), stop=(k == KD - 1))
                return ins
            sc.op("pe", fr, reads=["wr"] + [q[1] for q in xTt], writes=[PSK[6]])
            lg, lgk = get_tmp()
            sc.op("dve", lambda e, lg=lg: e.tensor_copy(out=lg[0:NE, 0:128], in_=ps[6][0:NE, 0:128]), reads=[PSK[6]], writes=[lgk])
            sc.op("sp", lambda e, lg=lg, t=t: e.dma_start(out=LOGT[:, t * 128:(t + 1) * 128], in_=lg[0:NE, 0:128]),
                  reads=[lgk], writes=["LOGT"], dma="lg_%s" % str(lgk))

    sc.barrier()
    es_p2.close()
    es_p3 = es.enter_context(ExitStack())
    scope[0] = es_p3
    lT = sb("lT", [NE, S])
    ex = sb("ex", [NE, S])
    aff = sb("aff", [NE, S])
    rsb = sb("rsb", [NE, 512])
    vals = sb("vals", [NE, CAP])
    idxu = sb("idxu", [NE, CAP], U32)
    idxf = sb("idxf", [NE, CAP])
    idxT = sb("idxT", [128, 2 * NE], U32)
    xgt = [sb("xgt%d" % i, [128, D], BF16) for i in range(3)]
    sc.op("sp", lambda e: e.dma_start(out=lT[:], in_=LOGT), reads=["LOGT"], writes=["lT"], dma="lT_ld")
    sc.op("act", lambda e: e.activation(out=ex[:], in_=lT[:], func=AF.Exp), reads=["lT"], writes=["ex"])
    for blk in range(4):
        sl = slice(blk * 512, (blk + 1) * 512)
        sc.op("pe", lambda e, blk=blk, sl=sl: e.matmul(ps[blk][0:NE, :], lhsT=onesf[0:NE, 0:NE], rhs=ex[:, sl], start=True, stop=True),
              reads=["ex", "onesf"], writes=[PSK[blk]])
        sc.op("dve", lambda e, blk=blk: e.reciprocal(out=rsb[:], in_=ps[blk][0:NE, :]), reads=[PSK[blk]], writes=["rsb"])
        sc.op("dve", lambda e, sl=sl: e.tensor_tensor(out=aff[:, sl], in0=ex[:, sl], in1=rsb[:], op=ALU.mult), reads=["ex", "rsb"], writes=["aff"])
    for it in range(CAP // 8):
        c8 = slice(it * 8, it * 8 + 8)
        sc.op("dve", lambda e, c8=c8: e.max(out=vals[:, c8], in_=aff[:]), reads=["aff"], writes=["vals"])
        sc.op("dve", lambda e, c8=c8: e.max_index(out=idxu[:, c8], in_max=vals[:, c8], in_values=aff[:]), reads=["aff", "vals"], writes=["idxu"])
        sc.op("dve", lambda e, c8=c8: e.match_replace(out=aff[:], in_to_replace=vals[:, c8], in_values=aff[:], imm_value=-1.0),
              reads=["vals", "aff"], writes=["aff"])
    sc.op("sp", lambda e: e.dma_start(out=idx_out, in_=idxu[:]), reads=["idxu"], dma="io_st")
    sc.op("sp", lambda e: e.dma_start(out=gate_out, in_=vals[:]), reads=["vals"], dma="io_st")
    sc.op("dve", lambda e: e.tensor_copy(out=idxf[:], in_=idxu[:]), reads=["idxu"], writes=["idxf"])
    for ct in range(2):
        sc.op("pe", lambda e, ct=ct: e.transpose(out=ps[4 + ct][:, 0:NE], in_=idxf[0:NE, ct * 128:(ct + 1) * 128], identity=identf[0:NE, 0:NE]),
              reads=["idxf", "identf"], writes=[PSK[4 + ct]])
        sc.op("dve", lambda e, ct=ct: e.tensor_copy(out=idxT[:, ct * NE:(ct + 1) * NE], in_=ps[4 + ct][:, 0:NE]), reads=[PSK[4 + ct]], writes=["idxT"])
    X1B_ALL = [("X1B", t) for t in range(NT)]
    n = 0
    for ex_ in range(NE):
        for ct in range(2):
            i = n % 3
            n += 1
            col = ct * NE + ex_
            sc.op("pool", lambda e, i=i, col=col: e.indirect_dma_start(
                out=xgt[i][:], out_offset=None, in_=X1B, in_offset=bass.IndirectOffsetOnAxis(ap=idxT[:, col:col + 1], axis=0)),
                reads=X1B_ALL + ["idxT"], writes=[("xgt", i)], dma="xgt%d" % i)
            r0 = ex_ * CAP + ct * 128
            sc.op("sp", lambda e, i=i, r0=r0: e.dma_start(out=xg_out[r0:r0 + 128, :], in_=xgt[i][:]), reads=[("xgt", i)], dma="xgst%d" % i)
    sc.final_wait("sp", ["ya_st", "io_st", "xgst0", "xgst1", "xgst2"] + (["dbg_st"] if debug else []))
    sc.emit()


def _rope_tables():
    f32 = np.float32
    pos = np.arange(S)
    row = (pos // 64).astype(f32)
    col = (pos % 64).astype(f32)
    invA = (f32(10000.0) ** (-np.arange(0, 64, 2, dtype=f32) / f32(64))).astype(f32)
    cA = np.ones((128, S), f32)
    sA = np.zeros((128, S), f32)
    permA = np.zeros((128, 128), f32)
    for p in range(128):
        posv = row if p < 64 else col
        j = p % 32
        ang = (posv * invA[j]).astype(f32)
        first = (p % 64) < 32
        cA[p] = np.cos(ang)
        sA[p] = -np.sin(ang) if first else np.sin(ang)
        partner = p + 32 if first else p - 32
        permA[partner, p] = 1.0
    invB = (f32(500000.0) ** (-np.arange(0, 32, 2, dtype=f32) / f32(32))).astype(f32)
    cB = np.ones((128, S), f32)
    sB = np.zeros((128, S), f32)
    permB = np.zeros((128, 128), f32)
    for p in range(128):
        if p < 32:
            j = p % 16
            ang = (pos.astype(f32) * invB[j]).astype(f32)
            first = p < 16
            cB[p] = np.cos(ang)
            sB[p] = -np.sin(ang) if first else np.sin(ang)
            partner = p + 16 if first else p - 16
        else:
            partner = p
        permB[partner, p] = 1.0
    return cA, sA, cB, sB, permA, permB


def _ln_tile(sc, zt_ap, zkey, dst, dkey, stats, mv, smalls, lng, lnb):
    for c in range(4):
        sc.op("dve", lambda e, c=c: e.bn_stats(out=stats[:, c, :], in_=zt_ap[:, c * 512:(c + 1) * 512]), reads=[zkey], writes=["stats"])
    sc.op("dve", lambda e: e.bn_aggr(out=mv[:, 0:2], in_=stats[:].rearrange("p a b -> p (a b)")), reads=["stats"], writes=["mv"])
    sc.op("act", lambda e: e.activation(out=mv[:, 2:3], in_=mv[:, 1:2], func=AF.Ln, bias=smalls[:, 6:7]), reads=["mv", "eps_ln"], writes=["mv2"])
    sc.op("act", lambda e: e.activation(out=mv[:, 2:3], in_=mv[:, 2:3], func=AF.Exp, scale=-0.5), reads=["mv2"], writes=["mv2"])
    sc.op("dve", lambda e: e.scalar_tensor_tensor(out=mv[:, 3:4], in0=mv[:, 0:1], scalar=-1.0, in1=mv[:, 2:3], op0=ALU.mult, op1=ALU.mult),
          reads=["mv", "mv2"], writes=["mv3"])
    sc.op("act", lambda e: e.activation(out=dst, in_=zt_ap, func=AF.Identity, scale=mv[:, 2:3], bias=mv[:, 3:4]), reads=[zkey, "mv2", "mv3"], writes=[dkey])
    sc.op("pool", lambda e: e.tensor_tensor(out=dst, in0=dst, in1=lng[:], op=ALU.mult), reads=[dkey, "lng"], writes=[dkey])
    sc.op("dve", lambda e: e.tensor_tensor(out=dst, in0=dst, in1=lnb[:], op=ALU.add), reads=[dkey, "lnb"], writes=[dkey])


def build_p2():
    nc = bass.Bass("TRN2", target_bir_lowering=False)
    with ExitStack() as es:
        NTOK = NCORE * CAP
        xg = nc.dram_tensor("xg", [2 * NTOK, D], BF16, kind="ExternalInput").ap()
        wg = nc.dram_tensor("wg", [2, D, FF], F32, kind="ExternalInput").ap()
        wu = nc.dram_tensor("wu", [2, D, FF], F32, kind="ExternalInput").ap()
        wd = nc.dram_tensor("wd", [2, FF, D], F32, kind="ExternalInput").ap()
        identb_d = nc.dram_tensor("identb", [128, 128], BF16, kind="ExternalInput").ap()
        o = nc.dram_tensor("o", [2 * NTOK, D], F32, kind="ExternalOutput").ap()
        sc = Sched(nc, es)
        sb = lambda name, shape, dt=F32: es.enter_context(nc.sbuf_tensor("s_" + name, list(shape), dt))
        ps = [es.enter_context(nc.psum_tensor("ps%d" % i, [128, 512], F32)) for i in range(8)]
        PSK = [("ps", i) for i in range(8)]
        identb = sb("identb", [128, 128], BF16)
        sc.op("sp", lambda e: e.dma_start(out=identb[:], in_=identb_d), writes=["identb"], dma="c_identb")
        HT = 1024
        xst = [sb("xst%d" % i, [128, D], BF16) for i in range(2)]
        xgT = sb("xgT", [128, KD, HT], BF16)
        hidT = sb("hidT", [128, FF // 128, HT], BF16)
        NW = 4
        wbuf = [sb("wbuf%d" % i, [128, 16, 512], BF16) for i in range(NW)]
        NTMP = 4
        tmp = [sb("tmp%d" % i, [128, 512]) for i in range(NTMP)]
        NOS = 4
        ostg = [sb("ostg%d" % i, [128, 512]) for i in range(NOS)]
        st = {"w": 0, "tmp": 0, "os": 0, "n": 0}

        def load_w(src3):
            i = st["w"] % NW
            st["w"] += 1
            sc.op("pool", lambda e, i=i, src3=src3: e.dma_start(out=wbuf[i][:], in_=src3), writes=[("w", i)], dma="w%d" % i)
            return wbuf[i], ("w", i)

        for el in range(2):
            for hh in range(2):
                base = el * NTOK + hh * HT
                for tt in range(HT // 128):
                    xi = tt % 2
                    r0 = base + tt * 128
                    sc.op("sp", lambda e, xi=xi, r0=r0: e.dma_start(out=xst[xi][:], in_=xg[r0:r0 + 128, :]), writes=[("xst", xi)], dma="xst%d" % xi)
                    for jb in range(2):
                        bank = jb

                        def tr(e, xi=xi, jb=jb, bank=bank):
                            pv = ps[bank][:].bitcast(BF16)
                            for i in range(8):
                                ins = e.transpose(out=pv[:, i * 128:(i + 1) * 128], in_=xst[xi][:, (jb * 8 + i) * 128:(jb * 8 + i + 1) * 128], identity=identb[:])
                            return ins
                        sc.op("pe", tr, reads=[("xst", xi), "identb"], writes=[PSK[bank]])
                        dst = xgT[:, jb * 8:(jb + 1) * 8, tt * 128:(tt + 1) * 128]
                        if jb == 0:
                            sc.op("act", lambda e, dst=dst, bank=bank: e.activation(out=dst, in_=ps[bank][:].bitcast(BF16).rearrange("p (a b) -> p a b", a=8), func=AF.Copy),
                                  reads=[PSK[bank]], writes=["xgT"])
                        else:
                            sc.op("dve", lambda e, dst=dst, bank=bank: e.tensor_copy(out=dst, in_=ps[bank][:].bitcast(BF16).rearrange("p (a b) -> p a b", a=8)),
                                  reads=[PSK[bank]], writes=["xgT"])
                for fg in range(FF // 512):
                    wgb, wgk = load_w(wg[el, :, fg * 512:(fg + 1) * 512].rearrange("(k p) c -> p k c", p=128))
                    wub, wuk = load_w(wu[el, :, fg * 512:(fg + 1) * 512].rearrange("(k p) c -> p k c", p=128))
                    for fi in range(4):
                        kf = fg * 4 + fi
                        for tb in range(HT // 512):
                            par = st["n"] % 2
                            st["n"] += 1
                            gb, ub = 2 + par, 4 + par

                            def fgate(e, wgb=wgb, fi=fi, tb=tb, gb=gb):
                                for k in range(KD):
                                    ins = e.matmul(ps[gb][:], lhsT=wgb[:, k, fi * 128:(fi + 1) * 128], rhs=xgT[:, k, tb * 512:(tb + 1) * 512], start=(k == 0), stop=(k == KD - 1))
                                return ins

                            def fup(e, wub=wub, fi=fi, tb=tb, ub=ub):
                                for k in range(KD):
                                    ins = e.matmul(ps[ub][:], lhsT=wub[:, k, fi * 128:(fi + 1) * 128], rhs=xgT[:, k, tb * 512:(tb + 1) * 512], start=(k == 0), stop=(k == KD - 1))
                                return ins
                            sc.op("pe", fgate, reads=[wgk, "xgT"], writes=[PSK[gb]])
                            sc.op("pe", fup, reads=[wuk, "xgT"], writes=[PSK[ub]])
                            ti = st["tmp"] % NTMP
                            st["tmp"] += 1
                            sc.op("act", lambda e, ti=ti, gb=gb: e.activation(out=tmp[ti][:], in_=ps[gb][:], func=AF.Silu), reads=[PSK[gb]], writes=[("tmp", ti)])
                            sc.op("dve", lambda e, ti=ti, ub=ub, kf=kf, tb=tb: e.tensor_tensor(out=hidT[:, kf, tb * 512:(tb + 1) * 512], in0=tmp[ti][:], in1=ps[ub][:], op=ALU.mult),
                                  reads=[("tmp", ti), PSK[ub]], writes=[("hid", kf)])
                HID = [("hid", kf) for kf in range(FF // 128)]
                for db in range(4):
                    wd0, wd0k = load_w(wd[el, 0:2048, db * 512:(db + 1) * 512].rearrange("(k p) c -> p k c", p=128))
                    wd1, wd1k = load_w(wd[el, 2048:4096, db * 512:(db + 1) * 512].rearrange("(k p) c -> p k c", p=128))
                    for tt in range(HT // 128):
                        bank = (6, 7, 0, 1)[st["n"] % 4]
                        st["n"] += 1

                        def fdown(e, wd0=wd0, wd1=wd1, tt=tt, bank=bank):
                            for k in range(32):
                                wsrc = wd0 if k < 16 else wd1
                                ins = e.matmul(ps[bank][:], lhsT=hidT[:, k, tt * 128:(tt + 1) * 128], rhs=wsrc[:, k % 16, :], start=(k == 0), stop=(k == 31))
                            return ins
                        sc.op("pe", fdown, reads=[wd0k, wd1k] + HID, writes=[PSK[bank]])
                        oi = st["os"] % NOS
                        st["os"] += 1
                        if oi % 2 == 0:
                            sc.op("act", lambda e, oi=oi, bank=bank: e.activation(out=ostg[oi][:], in_=ps[bank][:], func=AF.Copy), reads=[PSK[bank]], writes=[("os", oi)])
                        else:
                            sc.op("dve", lambda e, oi=oi, bank=bank: e.tensor_copy(out=ostg[oi][:], in_=ps[bank][:]), reads=[PSK[bank]], writes=[("os", oi)])
                        r0 = base + tt * 128
                        sc.op("sp", lambda e, oi=oi, r0=r0, db=db: e.dma_start(out=o[r0:r0 + 128, db * 512:(db + 1) * 512], in_=ostg[oi][:]),
                              reads=[("os", oi)], dma="ost%d" % oi)
        sc.final_wait("sp", ["ost%d" % i for i in range(NOS)])
        sc.emit()
    return nc


def build_p3():
    nc = bass.Bass("TRN2", target_bir_lowering=False)
    with ExitStack() as es:
        y_in = nc.dram_tensor("y_in", [S, D], F32, kind="ExternalInput").ap()
        og = nc.dram_tensor("og", [NE * CAP, D], F32, kind="ExternalInput").ap()
        idxT_d = nc.dram_tensor("idxT", [128, 2 * NE], U32, kind="ExternalInput").ap()
        gateT_d = nc.dram_tensor("gateT", [128, 2 * NE], F32, kind="ExternalInput").ap()
        ln2g_d = nc.dram_tensor("ln2g", [128, D], F32, kind="ExternalInput").ap()
        ln2b_d = nc.dram_tensor("ln2b", [128, D], F32, kind="ExternalInput").ap()
        out = nc.dram_tensor("out", [S, D], F32, kind="ExternalOutput").ap()
        YW = nc.dram_tensor("YW", [S, D], F32).ap()
        sc = Sched(nc, es)
        sb = lambda name, shape, dt=F32: es.enter_context(nc.sbuf_tensor("s_" + name, list(shape), dt))
        idxT = sb("idxT", [128, 2 * NE], U32)
        gateT = sb("gateT", [128, 2 * NE])
        lng = sb("lng", [128, D])
        lnb = sb("lnb", [128, D])
        smalls = sb("smalls", [128, 8])
        stats = sb("stats", [128, 4, 6])
        mv = sb("mv", [128, 8])
        ot = [sb("ot%d" % i, [128, D]) for i in range(3)]
        zt = [sb("z%d" % i, [128, D]) for i in range(2)]
        x1f = sb("x1f", [128, D])
        for dst, src, key in ((idxT, idxT_d, "idxT"), (gateT, gateT_d, "gateT"), (lng, ln2g_d, "lng"), (lnb, ln2b_d, "lnb")):
            sc.op("sp", lambda e, dst=dst, src=src: e.dma_start(out=dst[:], in_=src), writes=[key], dma="c_" + key)
        sc.op("pool", lambda e: e.memset(smalls[:, 6:7], LN_EPS), writes=["eps_ln"])
        sc.op("pool", lambda e: e.dma_start(out=YW, in_=y_in), writes=["YW"], dma="yw")
        for j in range(2 * NE):
            i = j % 3
            sc.op("sp", lambda e, i=i, j=j: e.dma_start(out=ot[i][:], in_=og[j * 128:(j + 1) * 128, :]), writes=[("ot", i)], dma="ot%d" % i)
            sc.op("dve", lambda e, i=i, j=j: e.tensor_scalar(out=ot[i][:], in0=ot[i][:], scalar1=gateT[:, j:j + 1], scalar2=None, op0=ALU.mult),
                  reads=[("ot", i), "gateT"], writes=[("ot", i)])
            sc.op("pool", lambda e, i=i, j=j: e.indirect_dma_start(
                out=YW, out_offset=bass.IndirectOffsetOnAxis(ap=idxT[:, j:j + 1], axis=0), in_=ot[i][:], in_offset=None, compute_op=ALU.add),
                reads=[("ot", i), "idxT"], writes=["YW"], dma="yw")
        for t in range(NT):
            zi = t % 2
            sc.op("sp", lambda e, zi=zi, t=t: e.dma_start(out=zt[zi][:], in_=YW[t * 128:(t + 1) * 128, :]), reads=["YW"], writes=[("z", zi)], dma="yld%d" % zi)
            _ln_tile(sc, zt[zi][:], ("z", zi), x1f[:], "x1f", stats, mv, smalls, lng, lnb)
            sc.op("sp", lambda e, t=t: e.dma_start(out=out[t * 128:(t + 1) * 128, :], in_=x1f[:]), reads=["x1f"], dma="out_st")
        sc.final_wait("sp", ["out_st"])
        sc.emit()
    return nc


_NC_CACHE = {}


def kernel(x, w_in, b_gate, a_q_norm, a_k_norm, b_lambda, b_subln, w_a_proj, w_b_proj,
           w_o, ln1_g, ln1_b, w_router, w_gate, w_up, w_down, ln2_g, ln2_b):
    f32 = np.float32
    A = lambda a: np.ascontiguousarray(np.asarray(a, dtype=f32))
    x = A(x)
    for name, fn in (("p1", build_p1), ("p2", build_p2), ("p3", build_p3)):
        if name not in _NC_CACHE:
            _NC_CACHE[name] = fn()
    cA, sA, cB, sB, permA, permB = _rope_tables()
    rep = lambda v: np.ascontiguousarray(np.broadcast_to(A(v).reshape(1, -1), (128, A(v).size)))
    identb = np.eye(128, dtype=f32).astype(ml_dtypes.bfloat16)
    shared = {
        "w_in": A(w_in[0]),
        "bgT": np.ascontiguousarray(A(b_gate[0]).reshape(32, 128).T),
        "gq": A(a_q_norm[0]).reshape(128, 1),
        "gk": A(a_k_norm[0]).reshape(128, 1),
        "lamb": rep(b_lambda[0]),
        "subln": np.ascontiguousarray(A(b_subln[0]).reshape(2, 128).T),
        "w_a": A(w_a_proj[0]), "w_b": A(w_b_proj[0]), "w_o": A(w_o[0]),
        "ln1g": rep(ln1_g[0]), "ln1b": rep(ln1_b[0]),
        "w_r": A(w_router[0]),
        "ropeA_c": cA, "ropeA_s": sA, "ropeB_c": cB, "ropeB_s": sB,
        "permA": permA, "permB": permB,
        "identf": np.eye(128, dtype=f32), "identb": identb,
    }
    cores = list(range(NCORE))
    r1 = run_bass_kernel_spmd(_NC_CACHE["p1"], [dict(shared, x=x[c]) for c in cores], core_ids=cores).results
    xg_all = np.stack([np.asarray(r["xg_out"]) for r in r1], 0).reshape(NCORE, NCORE, 2, CAP, D)
    xg_p2 = np.ascontiguousarray(xg_all.transpose(1, 2, 0, 3, 4)).reshape(NCORE, 2 * NCORE * CAP, D)
    del xg_all
    wg_all, wu_all, wd_all = A(w_gate[0]), A(w_up[0]), A(w_down[0])
    r2 = run_bass_kernel_spmd(_NC_CACHE["p2"], [
        {"xg": xg_p2[c], "wg": wg_all[2 * c:2 * c + 2], "wu": wu_all[2 * c:2 * c + 2], "wd": wd_all[2 * c:2 * c + 2], "identb": identb}
        for c in cores], core_ids=cores).results
    o_all = np.stack([np.asarray(r["o"], dtype=f32) for r in r2], 0).reshape(NCORE, 2, NCORE, CAP, D)
    og_p3 = np.ascontiguousarray(o_all.transpose(2, 0, 1, 3, 4)).reshape(NCORE, NE * CAP, D)
    del o_all
    lay = lambda a: np.ascontiguousarray(np.asarray(a).reshape(NE, 2, 128).transpose(2, 0, 1).reshape(128, 2 * NE))
    ln2g, ln2b = rep(ln2_g[0]), rep(ln2_b[0])
    r3 = run_bass_kernel_spmd(_NC_CACHE["p3"], [
        {"y_in": np.asarray(r1[c]["y_out"], dtype=f32), "og": og_p3[c],
         "idxT": lay(np.asarray(r1[c]["idx_out"]).astype(np.uint32)), "gateT": lay(np.asarray(r1[c]["gate_out"], dtype=f32)),
         "ln2g": ln2g, "ln2b": ln2b} for c in cores], core_ids=cores).results
    return np.stack([np.asarray(r["out"], dtype=f32) for r in r3], axis=0)
```

```python
import math
from contextlib import ExitStack

import numpy as np
import ml_dtypes

import concourse.bass as bass
import concourse.mybir as mybir
from concourse.bass_utils import run_bass_kernel_spmd

F32 = mybir.dt.float32
BF16 = mybir.dt.bfloat16
U32 = mybir.dt.uint32
AF = mybir.ActivationFunctionType
ALU = mybir.AluOpType
AX = mybir.AxisListType

S = 2048
D = 2048
NT = 16
KD = 16
NCORE = 8
NE = 16
CAP = 256
FF = 4096
IN_COLS = 8704
ALPHA = 2.0 ** 0.25
LAM_INIT = 0.2
RMS_EPS = 1e-6
LN_EPS = 1e-5
SCALE = 128.0 ** -0.5


class Sched:
    ENG = ("pe", "act", "dve", "pool", "sp")

    def __init__(self, nc, es):
        self.nc = nc
        self.es = es
        self.prog = {e: [] for e in self.ENG}
        self.semh = {}
        self.cnt = {}
        for e in self.ENG:
            self.semh[e] = es.enter_context(nc.semaphore("sem_" + e))
            self.cnt[e] = 0
        self.waited = {e: {} for e in self.ENG}
        self.lastw = {}
        self.readers = {}

    def _slot(self, name):
        if name not in self.semh:
            self.semh[name] = self.es.enter_context(self.nc.semaphore("d_" + name))
            self.cnt[name] = 0
        return name

    def op(self, eng, fn, reads=(), writes=(), dma=None):
        deps = {}

        def add(tok):
            if tok is None:
                return
            name, val = tok
            if name == "pe" and eng == "pe":
                return
            if deps.get(name, 0) < val:
                deps[name] = val

        for k in reads:
            add(self.lastw.get(k))
        for k in writes:
            add(self.lastw.get(k))
            for t in self.readers.get(k, {}).items():
                add(t)
        for name, val in deps.items():
            if self.waited[eng].get(name, 0) >= val:
                continue
            self.waited[eng][name] = val
            sem = self.semh[name]
            self.prog[eng].append(lambda e, sem=sem, val=val: e.wait_ge(sem, val))
        if dma is not None:
            name = self._slot(dma)
            self.cnt[name] += 16
            inc = 16
        else:
            name = eng
            self.cnt[name] += 1
            inc = 1
        tok = (name, self.cnt[name])
        sem = self.semh[name]
        self.prog[eng].append(lambda e, fn=fn, sem=sem, inc=inc: fn(e).then_inc(sem, inc))
        for k in reads:
            self.readers.setdefault(k, {})[name] = tok[1]
        for k in writes:
            self.lastw[k] = tok
            self.readers[k] = {}
        return tok

    def barrier(self):
        for eng in self.ENG:
            for name, sem in self.semh.items():
                val = self.cnt[name]
                if val == 0 or self.waited[eng].get(name, 0) >= val:
                    continue
                self.waited[eng][name] = val
                self.prog[eng].append(lambda e, sem=sem, val=val: e.wait_ge(sem, val))

    def final_wait(self, eng, names):
        for name in names:
            val = self.cnt[name]
            sem = self.semh[name]
            self.prog[eng].append(lambda e, sem=sem, val=val: e.wait_ge(sem, val))

    def emit(self):
        nc = self.nc
        with nc.Block() as block:
            @block.tensor
            def _(e):
                for f in self.prog["pe"]:
                    f(e)

            @block.scalar
            def _(e):
                for f in self.prog["act"]:
                    f(e)

            @block.vector
            def _(e):
                for f in self.prog["dve"]:
                    f(e)

            @block.gpsimd
            def _(e):
                for f in self.prog["pool"]:
                    f(e)

            @block.sync
            def _(e):
                for f in self.prog["sp"]:
                    f(e)


def build_p1(debug=False):
    nc = bass.Bass("TRN2", target_bir_lowering=False)
    es = ExitStack()
    with es:
        _build(nc, es, debug)
    return nc


def _build(nc, es, debug):
    def din(name, shape, dt=F32):
        return nc.dram_tensor(name, list(shape), dt, kind="ExternalInput").ap()

    def dint(name, shape, dt):
        return nc.dram_tensor(name, list(shape), dt, kind="Internal").ap()

    x = din("x", [S, D])
    w_in = din("w_in", [D, IN_COLS])
    bgT = din("bgT", [128, 32])
    gq_d = din("gq", [128, 1])
    gk_d = din("gk", [128, 1])
    lamb_d = din("lamb", [128, 512])
    subln_d = din("subln", [128, 2])
    w_a = din("w_a", [1024, D])
    w_b = din("w_b", [1024, D])
    w_o = din("w_o", [D, D])
    ln1g_d = din("ln1g", [128, D])
    ln1b_d = din("ln1b", [128, D])
    w_r = din("w_r", [D, NE])
    ropeA_c = din("ropeA_c", [128, S])
    ropeA_s = din("ropeA_s", [128, S])
    ropeB_c = din("ropeB_c", [128, S])
    ropeB_s = din("ropeB_s", [128, S])
    permA_d = din("permA", [128, 128])
    permB_d = din("permB", [128, 128])
    identf_d = din("identf", [128, 128])
    identb_d = din("identb", [128, 128], BF16)
    Y = nc.dram_tensor("y_out", [S, D], F32, kind="ExternalOutput").ap()
    xg_out = nc.dram_tensor("xg_out", [NE * CAP, D], BF16, kind="ExternalOutput").ap()
    idx_out = nc.dram_tensor("idx_out", [NE, CAP], U32, kind="ExternalOutput").ap()
    gate_out = nc.dram_tensor("gate_out", [NE, CAP], F32, kind="ExternalOutput").ap()
    if debug:
        dbg = nc.dram_tensor("dbg", [S, D], F32, kind="ExternalOutput").ap()
        dbg_ot = nc.dram_tensor("dbg_ot", [16 * 128, S], BF16, kind="ExternalOutput").ap()
        dbg_g = nc.dram_tensor("dbg_g", [32 * 128, S], F32, kind="ExternalOutput").ap()
        dbg_z = nc.dram_tensor("dbg_z", [S, D], F32, kind="ExternalOutput").ap()

    OT = dint("OT", [16 * 128, S], BF16)
    GATES = dint("GATES", [32 * 128, S], F32)
    X1B = dint("X1B", [S, D], BF16)
    LOGT = dint("LOGT", [NE, S], F32)

    sc = Sched(nc, es)
    scope = [es]
    sb = lambda name, shape, dt=F32: scope[0].enter_context(nc.sbuf_tensor("s_" + name, list(shape), dt))
    ps = [es.enter_context(nc.psum_tensor("ps%d" % i, [128, 512], F32)) for i in range(8)]
    PSK = [("ps", i) for i in range(8)]

    identf = sb("identf", [128, 128])
    identb = sb("identb", [128, 128], BF16)
    permA = sb("permA", [128, 128])
    permB = sb("permB", [128, 128])
    onesf = sb("onesf", [128, 128])
    onesb = sb("onesb", [128, 128], BF16)
    bg = sb("bg", [128, 32])
    gq = sb("gqs", [128, 1])
    gk = sb("gks", [128, 1])
    lamb = sb("lambs", [128, 512])
    subg = sb("subg", [128, 2])
    neglam = sb("neglam", [128, 1])
    smalls = sb("smalls", [128, 8])
    wr_sb = sb("wr_sb", [128, KD, NE])

    def ld(dst, src, key, q="sp"):
        sc.op(q, lambda e: e.dma_start(out=dst, in_=src), writes=[key], dma="c_" + key)

    ld(identf[:], identf_d, "identf")
    ld(identb[:], identb_d, "identb")
    ld(permA[:], permA_d, "permA")
    ld(permB[:], permB_d, "permB")
    ld(bg[:], bgT, "bg")
    ld(gq[:], gq_d, "gq")
    ld(gk[:], gk_d, "gk")
    ld(lamb[:], lamb_d, "lamb")
    ld(subg[:], subln_d, "subg")
    ld(wr_sb[:], w_r.rearrange("(k p) e -> p k e", p=128), "wr")
    sc.op("pool", lambda e: e.memset(onesf[:], 1.0), writes=["onesf"])
    sc.op("pool", lambda e: e.memset(onesb[:], 1.0), writes=["onesb"])
    lt = sb("lam_t", [128, 256])
    sc.op("dve", lambda e: e.tensor_tensor(out=lt[:, 0:128], in0=lamb[:, 0:128], in1=lamb[:, 128:256], op=ALU.mult),
          reads=["lamb"], writes=["lt"])
    sc.op("dve", lambda e: e.tensor_tensor(out=lt[:, 128:256], in0=lamb[:, 256:384], in1=lamb[:, 384:512], op=ALU.mult),
          reads=["lamb"], writes=["lt"])
    sc.op("dve", lambda e: e.reduce_sum(out=smalls[:, 0:1], in_=lt[:, 0:128], axis=AX.X), reads=["lt"], writes=["sm0"])
    sc.op("dve", lambda e: e.reduce_sum(out=smalls[:, 1:2], in_=lt[:, 128:256], axis=AX.X), reads=["lt"], writes=["sm1"])
    sc.op("act", lambda e: e.activation(out=smalls[:, 2:4], in_=smalls[:, 0:2], func=AF.Exp), reads=["sm0", "sm1"], writes=["sm2"])
    sc.op("dve", lambda e: e.tensor_tensor(out=smalls[:, 4:5], in0=smalls[:, 3:4], in1=smalls[:, 2:3], op=ALU.subtract),
          reads=["sm2"], writes=["sm4"])
    sc.op("dve", lambda e: e.tensor_scalar(out=neglam[:], in0=smalls[:, 4:5], scalar1=-LAM_INIT, scalar2=None, op0=ALU.add),
          reads=["sm4"], writes=["neglam"])
    sc.op("dve", lambda e: e.tensor_scalar(out=subg[:], in0=subg[:], scalar1=1.0 - LAM_INIT, scalar2=None, op0=ALU.mult),
          reads=["subg"], writes=["subg"])

    xT = sb("xT", [128, KD, S], BF16)
    NW = 3
    wbuf = [sb("wbuf%d" % i, [128, 16, 512], BF16) for i in range(NW)]
    xs = [sb("xs%d" % i, [128, D]) for i in range(2)]
    NTMP = 8
    tmp = [sb("tmp%d" % i, [128, 512]) for i in range(NTMP)]
    es_p1 = ExitStack()
    scope[0] = es_p1
    Qb = sb("Qb", [128, 4, S], BF16)
    Kb = sb("Kb", [128, 2, S], BF16)
    Vb = sb("Vb", [128, NT, 256], BF16)
    NPB = 4
    pT = [sb("pT%d" % i, [128, 512], BF16) for i in range(NPB)]
    ost = [sb("ost%d" % i, [128, 512], BF16) for i in range(2)]
    o0 = [sb("o0_%d" % i, [128, 512]) for i in range(2)]
    o1 = [sb("o1_%d" % i, [128, 512]) for i in range(2)]
    rc = [sb("rc%d" % i, [128, 512]) for i in range(2)]
    rs_ = [sb("rs%d" % i, [128, 512]) for i in range(2)]

    state = {"w": 0, "tmp": 0, "pT": 0, "ost": 0}

    def load_w(src3, nk, ncols, pieces=None):
        i = state["w"] % NW
        state["w"] += 1
        key = ("w", i)
        if pieces is None:
            pieces = [(0, src3)]
        for (k0, srcp) in pieces:
            nkp = srcp.shape[1]
            sc.op("pool", lambda e, srcp=srcp, k0=k0, nkp=nkp: e.dma_start(out=wbuf[i][:, k0:k0 + nkp, 0:ncols], in_=srcp),
                  writes=[key], dma="w%d" % i)
        return wbuf[i], key

    def get_tmp():
        i = state["tmp"] % NTMP
        state["tmp"] += 1
        return tmp[i], ("tmp", i)

    def wslab(w2d, c0, ncols):
        return w2d[:, c0:c0 + ncols].rearrange("(k p) c -> p k c", p=128)

    for t in range(NT):
        xst = xs[t % 2]
        xk = ("xs", t % 2)
        sc.op("sp", lambda e, xst=xst, t=t: e.dma_start(out=xst[:], in_=x[t * 128:(t + 1) * 128, :]),
              writes=[xk], dma="xs%d" % (t % 2))
        for j in range(4):
            bank = (t * 4 + j) % 4

            def tr(e, xst=xst, j=j, bank=bank):
                for i in range(4):
                    ins = e.transpose(out=ps[bank][:, i * 128:(i + 1) * 128],
                                      in_=xst[:, (4 * j + i) * 128:(4 * j + i + 1) * 128], identity=identf[:])
                return ins
            sc.op("pe", tr, reads=[xk, "identf"], writes=[PSK[bank]])
            eng = "act" if j % 2 == 0 else "dve"
            dst = xT[:, 4 * j:4 * j + 4, t * 128:(t + 1) * 128]
            srcv = ps[bank][:].rearrange("p (a b) -> p a b", a=4)
            if eng == "act":
                sc.op("act", lambda e, dst=dst, srcv=srcv: e.activation(out=dst, in_=srcv, func=AF.Copy),
                      reads=[PSK[bank]], writes=[("xT", t)])
            else:
                sc.op("dve", lambda e, dst=dst, srcv=srcv: e.tensor_copy(out=dst, in_=srcv),
                      reads=[PSK[bank]], writes=[("xT", t)])
    XT_ALL = [("xT", t) for t in range(NT)]

    def inproj_fm(wb, wkey, coff, blk, bank):
        def f(e):
            for k in range(KD):
                ins = e.matmul(ps[bank][:], lhsT=wb[:, k, coff:coff + 128], rhs=xT[:, k, blk * 512:(blk + 1) * 512],
                               start=(k == 0), stop=(k == KD - 1))
            return ins
        sc.op("pe", f, reads=[wkey] + [("xT", t) for t in range(blk * 4, blk * 4 + 4)], writes=[PSK[bank]])

    def load_rope(blk, which):
        i = blk % 2
        c_d, s_d = (ropeA_c, ropeA_s) if which == "A" else (ropeB_c, ropeB_s)
        sc.op("sp", lambda e: e.dma_start(out=rc[i][:], in_=c_d[:, blk * 512:(blk + 1) * 512]), writes=[("rc", i)], dma="rc%d" % i)
        sc.op("sp", lambda e: e.dma_start(out=rs_[i][:], in_=s_d[:, blk * 512:(blk + 1) * 512]), writes=[("rs", i)], dma="rs%d" % i)
        return i

    def rope_chunk(bank, bank2, bank3, ri, dst, dkey, gain, perm, permkey, rms):
        qg, qgk = get_tmp()
        if gain is not None:
            sc.op("act", lambda e: e.activation(out=qg[:], in_=ps[bank][:], func=AF.Copy, scale=gain[:]),
                  reads=[PSK[bank], "gq", "gk"], writes=[qgk])
        else:
            sc.op("act", lambda e: e.activation(out=qg[:], in_=ps[bank][:], func=AF.Copy),
                  reads=[PSK[bank]], writes=[qgk])
        if rms:
            sq, sqk = get_tmp()
            sc.op("act", lambda e: e.activation(out=sq[:], in_=ps[bank][:], func=AF.Square),
                  reads=[PSK[bank]], writes=[sqk])
            sc.op("pe", lambda e: e.matmul(ps[bank2][:], lhsT=onesf[:], rhs=sq[:], start=True, stop=True),
                  reads=[sqk, "onesf"], writes=[PSK[bank2]])
            rstd, rk = get_tmp()
            sc.op("act", lambda e: e.activation(out=rstd[:], in_=ps[bank2][:], func=AF.Ln, scale=1.0 / 128.0, bias=smalls[:, 5:6]),
                  reads=[PSK[bank2], "eps_rms"], writes=[rk])
            sc.op("act", lambda e: e.activation(out=rstd[:], in_=rstd[:], func=AF.Exp, scale=-0.5),
                  reads=[rk], writes=[rk])
        sc.op("pe", lambda e: e.matmul(ps[bank3][:], lhsT=perm[:], rhs=qg[:], start=True, stop=True),
              reads=[qgk, permkey], writes=[PSK[bank3]])
        t1, t1k = get_tmp()
        sc.op("pool", lambda e: e.tensor_tensor(out=t1[:], in0=qg[:], in1=rc[ri][:], op=ALU.mult),
              reads=[qgk, ("rc", ri)], writes=[t1k])
        t2, t2k = get_tmp()
        sc.op("dve", lambda e: e.tensor_tensor(out=t2[:], in0=ps[bank3][:], in1=rs_[ri][:], op=ALU.mult),
              reads=[PSK[bank3], ("rs", ri)], writes=[t2k])
        if rms:
            sc.op("dve", lambda e: e.tensor_tensor(out=t1[:], in0=t1[:], in1=t2[:], op=ALU.add),
                  reads=[t1k, t2k], writes=[t1k])
            sc.op("dve", lambda e: e.tensor_tensor(out=dst, in0=t1[:], in1=rstd[:], op=ALU.mult),
                  reads=[t1k, rk], writes=[dkey])
        else:
            sc.op("dve", lambda e: e.tensor_tensor(out=dst, in0=t1[:], in1=t2[:], op=ALU.add),
                  reads=[t1k, t2k], writes=[dkey])

    sc.op("pool", lambda e: e.memset(smalls[:, 5:6], RMS_EPS), writes=["eps_rms"])
    sc.op("pool", lambda e: e.memset(smalls[:, 6:7], LN_EPS), writes=["eps_ln"])

    def inproj_v(wb, wkey, dv):
        per = 512 // dv
        for t0 in range(0, NT, per):
            bank = 4 + (t0 // per) % 2

            def f(e, t0=t0, bank=bank):
                for i in range(per):
                    t = t0 + i
                    for k in range(KD):
                        ins = e.matmul(ps[bank][:, i * dv:(i + 1) * dv], lhsT=xT[:, k, t * 128:(t + 1) * 128],
                                       rhs=wb[:, k, 0:dv], start=(k == 0), stop=(k == KD - 1))
                return ins
            sc.op("pe", f, reads=[wkey] + XT_ALL[t0:t0 + per], writes=[PSK[bank]])
            srcv = ps[bank][:].rearrange("p (a b) -> p a b", a=per)
            sc.op("act", lambda e, t0=t0, srcv=srcv: e.activation(out=Vb[:, t0:t0 + per, 0:dv], in_=srcv, func=AF.Copy),
                  reads=[PSK[bank]], writes=["V"])

    unit_no = [0]

    def attention_unit(qi, ki, blk, dv, epilogue):
        u = unit_no[0] % 2
        unit_no[0] += 1
        nh = dv // 128
        obank = [2 + 2 * u, 3 + 2 * u]
        lbank = 6 + u
        qap = Qb[:, qi, blk * 512:(blk + 1) * 512]
        pk = [None] * NT

        def qk(kt):
            sb_ = kt % 2
            sc.op("pe", lambda e: e.matmul(ps[sb_][:], lhsT=Kb[:, ki, kt * 128:(kt + 1) * 128], rhs=qap, start=True, stop=True),
                  reads=[("Q", qi), ("K", ki)], writes=[PSK[sb_]])
            i = state["pT"] % NPB
            state["pT"] += 1
            pk[kt] = i
            sc.op("act", lambda e: e.activation(out=pT[i][:], in_=ps[sb_][:], func=AF.Exp, scale=SCALE),
                  reads=[PSK[sb_]], writes=[("pT", i)])

        def pv(kt):
            i = pk[kt]

            def f(e):
                for h in range(nh):
                    e.matmul(ps[obank[h]][:], lhsT=Vb[:, kt, h * 128:(h + 1) * 128], rhs=pT[i][:],
                             start=(kt == 0), stop=(kt == NT - 1))
                return e.matmul(ps[lbank][:], lhsT=onesb[:], rhs=pT[i][:], start=(kt == 0), stop=(kt == NT - 1))
            sc.op("pe", f, reads=[("pT", i), "V", "onesb"], writes=[PSK[obank[h]] for h in range(nh)] + [PSK[lbank]])

        qk(0)
        qk(1)
        for kt in range(NT):
            pv(kt)
            if kt + 2 < NT:
                qk(kt + 2)
        rl, rlk = get_tmp()
        sc.op("dve", lambda e: e.reciprocal(out=rl[:], in_=ps[lbank][:]), reads=[PSK[lbank]], writes=[rlk])
        epilogue(obank, rl, rlk)

    def store_oT(src_tile, skey, chunk, blk):
        sc.op("sp", lambda e: e.dma_start(out=OT[chunk * 128:(chunk + 1) * 128, blk * 512:(blk + 1) * 512], in_=src_tile[:]),
              reads=[skey], dma="ost_%s" % str(skey))

    for hk in range(2):
        wq, wqk = load_w(wslab(w_in, hk * 512, 512), KD, 512)
        wk_, wkk = load_w(wslab(w_in, 1024 + hk * 128, 128), KD, 128)
        wv_, wvk = load_w(wslab(w_in, 1280 + hk * 128, 128), KD, 128)
        for blk in range(4):
            ri = load_rope(blk, "A")
            for g in range(4):
                inproj_fm(wq, wqk, g * 128, blk, 0)
                rope_chunk(0, 1, 2, ri, Qb[:, g, blk * 512:(blk + 1) * 512], ("Q", g), gq, permA, "permA", True)
            inproj_fm(wk_, wkk, 0, blk, 0)
            rope_chunk(0, 1, 2, ri, Kb[:, 0, blk * 512:(blk + 1) * 512], ("K", 0), gk, permA, "permA", True)
        inproj_v(wv_, wvk, 128)
        for g in range(4):
            for blk in range(4):
                def epiA(obank, rl, rlk, g=g, blk=blk):
                    i = state["ost"] % 2
                    state["ost"] += 1
                    sc.op("dve", lambda e: e.tensor_tensor(out=ost[i][:], in0=ps[obank[0]][:], in1=rl[:], op=ALU.mult),
                          reads=[PSK[obank[0]], rlk], writes=[("ost", i)])
                    store_oT(ost[i], ("ost", i), hk * 4 + g, blk)
                attention_unit(g, 0, blk, 128, epiA)

    for h in range(4):
        wq, wqk = load_w(wslab(w_in, 1536 + h * 256, 256), KD, 256)
        wk_, wkk = load_w(wslab(w_in, 2560 + h * 256, 256), KD, 256)
        wv_, wvk = load_w(wslab(w_in, 3584 + h * 256, 256), KD, 256)
        for blk in range(4):
            ri = load_rope(blk, "B")
            for c in range(2):
                inproj_fm(wq, wqk, c * 128, blk, 0)
                rope_chunk(0, 1, 2, ri, Qb[:, c, blk * 512:(blk + 1) * 512], ("Q", c), None, permB, "permB", False)
                inproj_fm(wk_, wkk, c * 128, blk, 0)
                rope_chunk(0, 1, 2, ri, Kb[:, c, blk * 512:(blk + 1) * 512], ("K", c), None, permB, "permB", False)
        inproj_v(wv_, wvk, 256)
        for blk in range(4):
            def epiB0(obank, rl, rlk):
                for hf in range(2):
                    sc.op("dve", lambda e, hf=hf: e.tensor_tensor(out=o0[hf][:], in0=ps[obank[hf]][:], in1=rl[:], op=ALU.mult),
                          reads=[PSK[obank[hf]], rlk], writes=[("o0", hf)])

            def epiB1(obank, rl, rlk, h=h, blk=blk):
                sqs = []
                for hf in range(2):
                    sc.op("dve", lambda e, hf=hf: e.tensor_tensor(out=o1[hf][:], in0=ps[obank[hf]][:], in1=rl[:], op=ALU.mult),
                          reads=[PSK[obank[hf]], rlk], writes=[("o1", hf)])
                    sc.op("dve", lambda e, hf=hf: e.scalar_tensor_tensor(out=o1[hf][:], in0=o1[hf][:], scalar=neglam[:], in1=o0[hf][:],
                                                                      op0=ALU.mult, op1=ALU.add),
                          reads=[("o1", hf), ("o0", hf), "neglam"], writes=[("o1", hf)])
                    sq, sqk = get_tmp()
                    sc.op("act", lambda e, hf=hf, sq=sq: e.activation(out=sq[:], in_=o1[hf][:], func=AF.Square),
                          reads=[("o1", hf)], writes=[sqk])
                    sqs.append((sq, sqk))
                lb = obank[0]

                def f(e):
                    e.matmul(ps[lb][:], lhsT=onesf[:], rhs=sqs[0][0][:], start=True, stop=False)
                    return e.matmul(ps[lb][:], lhsT=onesf[:], rhs=sqs[1][0][:], start=False, stop=True)
                sc.op("pe", f, reads=[sqs[0][1], sqs[1][1], "onesf"], writes=[PSK[lb]])
                rstd, rk = get_tmp()
                sc.op("act", lambda e: e.activation(out=rstd[:], in_=ps[lb][:], func=AF.Ln, scale=1.0 / 256.0, bias=smalls[:, 5:6]),
                      reads=[PSK[lb], "eps_rms"], writes=[rk])
                sc.op("act", lambda e: e.activation(out=rstd[:], in_=rstd[:], func=AF.Exp, scale=-0.5), reads=[rk], writes=[rk])
                for hf in range(2):
                    i = state["ost"] % 2
                    state["ost"] += 1
                    sc.op("dve", lambda e, hf=hf, i=i: e.scalar_tensor_tensor(out=ost[i][:], in0=o1[hf][:], scalar=subg[:, hf:hf + 1], in1=rstd[:],
                                                                           op0=ALU.mult, op1=ALU.mult),
                          reads=[("o1", hf), "subg", rk], writes=[("ost", i)])
                    store_oT(ost[i], ("ost", i), 8 + h * 2 + hf, blk)
            attention_unit(0, 0, blk, 256, epiB0)
            attention_unit(1, 1, blk, 256, epiB1)

    for cg in range(8):
        wgt, wgk = load_w(wslab(w_in, 4608 + cg * 512, 512), KD, 512)
        for jj in range(4):
            j = cg * 4 + jj
            for blk in range(4):
                bank = (jj * 4 + blk) % 4
                inproj_fm(wgt, wgk, jj * 128, blk, bank)
                tt_, tk = get_tmp()
                sc.op("act", lambda e, tt_=tt_, bank=bank, j=j: e.activation(out=tt_[:], in_=ps[bank][:], func=AF.Sigmoid, bias=bg[:, j:j + 1]),
                      reads=[PSK[bank], "bg"], writes=[tk])
                sc.op("sp", lambda e, tt_=tt_, j=j, blk=blk: e.dma_start(out=GATES[j * 128:(j + 1) * 128, blk * 512:(blk + 1) * 512], in_=tt_[:]),
                      reads=[tk], writes=[("GATES", j, blk)], dma="gst_%s" % str(tk))

    sc.barrier()
    es_p1.close()
    if debug:
        sc.op("sp", lambda e: e.dma_start(out=dbg_ot, in_=OT), dma="dbg_st")
        sc.op("sp", lambda e: e.dma_start(out=dbg_g, in_=GATES), dma="dbg_st")
    es_p2 = es.enter_context(ExitStack())
    scope[0] = es_p2
    oTg = [xT[:, 0:16, 0:512], xT[:, 0:16, 512:1024]]
    mrg = xT[:, 0:16, 1024:1536]
    zt = [sb("z%d" % i, [128, D]) for i in range(4)]
    x1f = xs[0]
    x1b = sb("x1b", [128, D], BF16)
    ya_t = xs[1]
    lng = sb("lng", [128, D])
    lnb = sb("lnb", [128, D])
    stats = sb("stats", [128, 4, 6])
    mv = sb("mv", [128, 8])
    ld(lng[:], ln1g_d, "lng")
    ld(lnb[:], ln1b_d, "lnb")

    for g in range(4):
        og = oTg[g % 2]
        ogk = ("oTg", g % 2)
        sc.op("sp", lambda e, og=og, g=g: e.dma_start(out=og, in_=OT[:, g * 512:(g + 1) * 512].rearrange("(k p) s -> p k s", p=128)),
              writes=[ogk], dma="oTg%d" % (g % 2))
        for jj in range(8):
            wab, wabk = load_w(None, 0, 256, pieces=[(0, w_a[:, jj * 256:(jj + 1) * 256].rearrange("(k p) c -> p k c", p=128)),
                                                     (8, w_b[:, jj * 256:(jj + 1) * 256].rearrange("(k p) c -> p k c", p=128))])
            for j2 in range(2):
                j = jj * 2 + j2

                def fa(e, j2=j2, og=og, wab=wab):
                    for k in range(8):
                        ins = e.matmul(ps[0][:], lhsT=wab[:, k, j2 * 128:(j2 + 1) * 128], rhs=og[:, k, :], start=(k == 0), stop=(k == 7))
                    return ins

                def fb(e, j2=j2, og=og, wab=wab):
                    for k in range(8):
                        ins = e.matmul(ps[1][:], lhsT=wab[:, 8 + k, j2 * 128:(j2 + 1) * 128], rhs=og[:, 8 + k, :], start=(k == 0), stop=(k == 7))
                    return ins
                sc.op("pe", fa, reads=[wabk, ogk], writes=[PSK[0]])
                sc.op("pe", fb, reads=[wabk, ogk], writes=[PSK[1]])
                g0, g0k = get_tmp()
                g1, g1k = get_tmp()
                sc.op("sp", lambda e, g0=g0, j=j, g=g: e.dma_start(out=g0[:], in_=GATES[j * 128:(j + 1) * 128, g * 512:(g + 1) * 512]),
                      reads=[("GATES", j, g)], writes=[g0k], dma="gld_%s" % str(g0k))
                sc.op("sp", lambda e, g1=g1, j=j, g=g: e.dma_start(out=g1[:], in_=GATES[(16 + j) * 128:(17 + j) * 128, g * 512:(g + 1) * 512]),
                      reads=[("GATES", 16 + j, g)], writes=[g1k], dma="gld_%s" % str(g1k))
                sc.op("dve", lambda e, g0=g0: e.tensor_tensor(out=g0[:], in0=ps[0][:], in1=g0[:], op=ALU.mult), reads=[PSK[0], g0k], writes=[g0k])
                sc.op("dve", lambda e, g1=g1: e.tensor_tensor(out=g1[:], in0=ps[1][:], in1=g1[:], op=ALU.mult), reads=[PSK[1], g1k], writes=[g1k])
                sc.op("dve", lambda e, g0=g0, g1=g1, j=j: e.tensor_tensor(out=mrg[:, j, :], in0=g0[:], in1=g1[:], op=ALU.add),
                      reads=[g0k, g1k], writes=[("mrg", j)])
        MRG = [("mrg", j) for j in range(16)]
        for eb in range(4):
            wob, wobk = load_w(wslab(w_o, eb * 512, 512), KD, 512)
            for tt in range(4):
                t = g * 4 + tt
                bank = 2 + (eb * 4 + tt) % 4

                def fo(e, tt=tt, bank=bank, wob=wob):
                    for k in range(KD):
                        ins = e.matmul(ps[bank][:], lhsT=mrg[:, k, tt * 128:(tt + 1) * 128], rhs=wob[:, k, :], start=(k == 0), stop=(k == KD - 1))
                    return ins
                sc.op("pe", fo, reads=[wobk] + MRG, writes=[PSK[bank]])
                xr, xrk = get_tmp()
                sc.op("sp", lambda e, xr=xr, t=t, eb=eb: e.dma_start(out=xr[:], in_=x[t * 128:(t + 1) * 128, eb * 512:(eb + 1) * 512]),
                      writes=[xrk], dma="xr_%s" % str(xrk))
                sc.op("dve", lambda e, xr=xr, tt=tt, eb=eb, bank=bank: e.scalar_tensor_tensor(
                    out=zt[tt][:, eb * 512:(eb + 1) * 512], in0=xr[:], scalar=ALPHA, in1=ps[bank][:], op0=ALU.mult, op1=ALU.add),
                    reads=[xrk, PSK[bank]], writes=[("z", tt)])
        for tt in range(4):
            t = g * 4 + tt
            if debug:
                sc.op("sp", lambda e, tt=tt, t=t: e.dma_start(out=dbg_z[t * 128:(t + 1) * 128, :], in_=zt[tt][:]),
                      reads=[("z", tt)], dma="dbg_st")
            _ln_tile(sc, zt[tt][:], ("z", tt), x1f[:], "x1f", stats, mv, smalls, lng, lnb)
            sc.op("act", lambda e: e.activation(out=x1b[:], in_=x1f[:], func=AF.Copy), reads=["x1f"], writes=["x1b"])
            sc.op("sp", lambda e, t=t: e.dma_start(out=X1B[t * 128:(t + 1) * 128, :], in_=x1b[:]), reads=["x1b"], writes=[("X1B", t)], dma="x1b_st")
            sc.op("act", lambda e: e.activation(out=ya_t[:], in_=x1f[:], func=AF.Copy, scale=ALPHA), reads=["x1f"], writes=["ya_t"])
            sc.op("sp", lambda e, t=t: e.dma_start(out=Y[t * 128:(t + 1) * 128, :], in_=ya_t[:]), reads=["ya_t"], writes=[("Y", t)], dma="ya_st")
            xTt = []
            for j in range(4):
                bank = j % 2
                xt_, xtk = get_tmp()
                xTt.append((xt_, xtk))

                def trr(e, j=j, bank=bank):
                    for i in range(4):
                        ins = e.transpose(out=ps[bank][:, i * 128:(i + 1) * 128], in_=x1f[:, (4 * j + i) * 128:(4 * j + i + 1) * 128], identity=identf[:])
                    return ins
                sc.op("pe", trr, reads=["x1f", "identf"], writes=[PSK[bank]])
                sc.op("act", lambda e, xt_=xt_, bank=bank: e.activation(out=xt_[:], in_=ps[bank][:], func=AF.Copy), reads=[PSK[bank]], writes=[xtk])

            def fr(e, xTt=xTt):
                for k in range(KD):
                    ins = e.matmul(ps[6][0:NE, 0:128], lhsT=wr_sb[:, k, :], rhs=xTt[k // 4][0][:, (k % 4) * 128:(k % 4 + 1) * 128],
                                   start=(k == 0), stop=(k == KD - 1))
                return ins
            sc.op("pe", fr, reads=["wr"] + [q[1] for q in xTt], writes=[PSK[6]])
            lg, lgk = get_tmp()
            sc.op("dve", lambda e, lg=lg: e.tensor_copy(out=lg[0:NE, 0:128], in_=ps[6][0:NE, 0:128]), reads=[PSK[6]], writes=[lgk])
            sc.op("sp", lambda e, lg=lg, t=t: e.dma_start(out=LOGT[:, t * 128:(t + 1) * 128], in_=lg[0:NE, 0:128]),
                  reads=[lgk], writes=["LOGT"], dma="lg_%s" % str(lgk))

    sc.barrier()
    es_p2.close()
    es_p3 = es.enter_context(ExitStack())
    scope[0] = es_p3
    lT = sb("lT", [NE, S])
    ex = sb("ex", [NE, S])
    aff = sb("aff", [NE, S])
    rsb = sb("rsb", [NE, 512])
    vals = sb("vals", [NE, CAP])
    idxu = sb("idxu", [NE, CAP], U32)
    idxf = sb("idxf", [NE, CAP])
    idxT = sb("idxT", [128, 2 * NE], U32)
    xgt = [sb("xgt%d" % i, [128, D], BF16) for i in range(3)]
    sc.op("sp", lambda e: e.dma_start(out=lT[:], in_=LOGT), reads=["LOGT"], writes=["lT"], dma="lT_ld")
    sc.op("act", lambda e: e.activation(out=ex[:], in_=lT[:], func=AF.Exp), reads=["lT"], writes=["ex"])
    for blk in range(4):
        sl = slice(blk * 512, (blk + 1) * 512)
        sc.op("pe", lambda e, blk=blk, sl=sl: e.matmul(ps[blk][0:NE, :], lhsT=onesf[0:NE, 0:NE], rhs=ex[:, sl], start=True, stop=True),
              reads=["ex", "onesf"], writes=[PSK[blk]])
        sc.op("dve", lambda e, blk=blk: e.reciprocal(out=rsb[:], in_=ps[blk][0:NE, :]), reads=[PSK[blk]], writes=["rsb"])
        sc.op("dve", lambda e, sl=sl: e.tensor_tensor(out=aff[:, sl], in0=ex[:, sl], in1=rsb[:], op=ALU.mult), reads=["ex", "rsb"], writes=["aff"])
    for it in range(CAP // 8):
        c8 = slice(it * 8, it * 8 + 8)
        sc.op("dve", lambda e, c8=c8: e.max(out=vals[:, c8], in_=aff[:]), reads=["aff"], writes=["vals"])
        sc.op("dve", lambda e, c8=c8: e.max_index(out=idxu[:, c8], in_max=vals[:, c8], in_values=aff[:]), reads=["aff", "vals"], writes=["idxu"])
        sc.op("dve", lambda e, c8=c8: e.match_replace(out=aff[:], in_to_replace=vals[:, c8], in_values=aff[:], imm_value=-1.0),
              reads=["vals", "aff"], writes=["aff"])
    sc.op("sp", lambda e: e.dma_start(out=idx_out, in_=idxu[:]), reads=["idxu"], dma="io_st")
    sc.op("sp", lambda e: e.dma_start(out=gate_out, in_=vals[:]), reads=["vals"], dma="io_st")
    sc.op("dve", lambda e: e.tensor_copy(out=idxf[:], in_=idxu[:]), reads=["idxu"], writes=["idxf"])
    for ct in range(2):
        sc.op("pe", lambda e, ct=ct: e.transpose(out=ps[4 + ct][:, 0:NE], in_=idxf[0:NE, ct * 128:(ct + 1) * 128], identity=identf[0:NE, 0:NE]),
              reads=["idxf", "identf"], writes=[PSK[4 + ct]])
        sc.op("dve", lambda e, ct=ct: e.tensor_copy(out=idxT[:, ct * NE:(ct + 1) * NE], in_=ps[4 + ct][:, 0:NE]), reads=[PSK[4 + ct]], writes=["idxT"])
    X1B_ALL = [("X1B", t) for t in range(NT)]
    n = 0
    for ex_ in range(NE):
        for ct in range(2):
            i = n % 3
            n += 1
            col = ct * NE + ex_
            sc.op("pool", lambda e, i=i, col=col: e.indirect_dma_start(
                out=xgt[i][:], out_offset=None, in_=X1B, in_offset=bass.IndirectOffsetOnAxis(ap=idxT[:, col:col + 1], axis=0)),
                reads=X1B_ALL + ["idxT"], writes=[("xgt", i)], dma="xgt%d" % i)
            r0 = ex_ * CAP + ct * 128
            sc.op("sp", lambda e, i=i, r0=r0: e.dma_start(out=xg_out[r0:r0 + 128, :], in_=xgt[i][:]), reads=[("xgt", i)], dma="xgst%d" % i)
    sc.final_wait("sp", ["ya_st", "io_st", "xgst0", "xgst1", "xgst2"] + (["dbg_st"] if debug else []))
    sc.emit()


def _rope_tables():
    f32 = np.float32
    pos = np.arange(S)
    row = (pos // 64).astype(f32)
    col = (pos % 64).astype(f32)
    invA = (f32(10000.0) ** (-np.arange(0, 64, 2, dtype=f32) / f32(64))).astype(f32)
    cA = np.ones((128, S), f32)
    sA = np.zeros((128, S), f32)
    permA = np.zeros((128, 128), f32)
    for p in range(128):
        posv = row if p < 64 else col
        j = p % 32
        ang = (posv * invA[j]).astype(f32)
        first = (p % 64) < 32
        cA[p] = np.cos(ang)
        sA[p] = -np.sin(ang) if first else np.sin(ang)
        partner = p + 32 if first else p - 32
        permA[partner, p] = 1.0
    invB = (f32(500000.0) ** (-np.arange(0, 32, 2, dtype=f32) / f32(32))).astype(f32)
    cB = np.ones((128, S), f32)
    sB = np.zeros((128, S), f32)
    permB = np.zeros((128, 128), f32)
    for p in range(128):
        if p < 32:
            j = p % 16
            ang = (pos.astype(f32) * invB[j]).astype(f32)
            first = p < 16
            cB[p] = np.cos(ang)
            sB[p] = -np.sin(ang) if first else np.sin(ang)
            partner = p + 16 if first else p - 16
        else:
            partner = p
        permB[partner, p] = 1.0
    return cA, sA, cB, sB, permA, permB


def _ln_tile(sc, zt_ap, zkey, dst, dkey, stats, mv, smalls, lng, lnb):
    for c in range(4):
        sc.op("dve", lambda e, c=c: e.bn_stats(out=stats[:, c, :], in_=zt_ap[:, c * 512:(c + 1) * 512]), reads=[zkey], writes=["stats"])
    sc.op("dve", lambda e: e.bn_aggr(out=mv[:, 0:2], in_=stats[:].rearrange("p a b -> p (a b)")), reads=["stats"], writes=["mv"])
    sc.op("act", lambda e: e.activation(out=mv[:, 2:3], in_=mv[:, 1:2], func=AF.Ln, bias=smalls[:, 6:7]), reads=["mv", "eps_ln"], writes=["mv2"])
    sc.op("act", lambda e: e.activation(out=mv[:, 2:3], in_=mv[:, 2:3], func=AF.Exp, scale=-0.5), reads=["mv2"], writes=["mv2"])
    sc.op("dve", lambda e: e.scalar_tensor_tensor(out=dst, in0=zt_ap, scalar=mv[:, 0:1], in1=lng[:], op0=ALU.subtract, op1=ALU.mult),
          reads=[zkey, "mv", "lng"], writes=[dkey])
    sc.op("dve", lambda e: e.scalar_tensor_tensor(out=dst, in0=dst, scalar=mv[:, 2:3], in1=lnb[:], op0=ALU.mult, op1=ALU.add),
          reads=[dkey, "mv2", "lnb"], writes=[dkey])


def build_p2():
    nc = bass.Bass("TRN2", target_bir_lowering=False)
    with ExitStack() as es:
        NTOK = NCORE * CAP
        xg = nc.dram_tensor("xg", [2 * NTOK, D], BF16, kind="ExternalInput").ap()
        wg = nc.dram_tensor("wg", [2, D, FF], F32, kind="ExternalInput").ap()
        wu = nc.dram_tensor("wu", [2, D, FF], F32, kind="ExternalInput").ap()
        wd = nc.dram_tensor("wd", [2, FF, D], F32, kind="ExternalInput").ap()
        identb_d = nc.dram_tensor("identb", [128, 128], BF16, kind="ExternalInput").ap()
        o = nc.dram_tensor("o", [2 * NTOK, D], F32, kind="ExternalOutput").ap()
        sc = Sched(nc, es)
        sb = lambda name, shape, dt=F32: es.enter_context(nc.sbuf_tensor("s_" + name, list(shape), dt))
        ps = [es.enter_context(nc.psum_tensor("ps%d" % i, [128, 512], F32)) for i in range(8)]
        PSK = [("ps", i) for i in range(8)]
        identb = sb("identb", [128, 128], BF16)
        sc.op("sp", lambda e: e.dma_start(out=identb[:], in_=identb_d), writes=["identb"], dma="c_identb")
        HT = 1024
        xst = [sb("xst%d" % i, [128, D], BF16) for i in range(2)]
        xgT = sb("xgT", [128, KD, HT], BF16)
        hidT = sb("hidT", [128, FF // 128, HT], BF16)
        NW = 4
        wbuf = [sb("wbuf%d" % i, [128, 16, 512], BF16) for i in range(NW)]
        NTMP = 4
        tmp = [sb("tmp%d" % i, [128, 512]) for i in range(NTMP)]
        NOS = 4
        ostg = [sb("ostg%d" % i, [128, 512]) for i in range(NOS)]
        st = {"w": 0, "tmp": 0, "os": 0, "n": 0}

        def load_w(src3):
            i = st["w"] % NW
            st["w"] += 1
            sc.op("pool", lambda e, i=i, src3=src3: e.dma_start(out=wbuf[i][:], in_=src3), writes=[("w", i)], dma="w%d" % i)
            return wbuf[i], ("w", i)

        for el in range(2):
            for hh in range(2):
                base = el * NTOK + hh * HT
                for tt in range(HT // 128):
                    xi = tt % 2
                    r0 = base + tt * 128
                    sc.op("sp", lambda e, xi=xi, r0=r0: e.dma_start(out=xst[xi][:], in_=xg[r0:r0 + 128, :]), writes=[("xst", xi)], dma="xst%d" % xi)
                    for jb in range(2):
                        bank = jb

                        def tr(e, xi=xi, jb=jb, bank=bank):
                            pv = ps[bank][:].bitcast(BF16)
                            for i in range(8):
                                ins = e.transpose(out=pv[:, i * 128:(i + 1) * 128], in_=xst[xi][:, (jb * 8 + i) * 128:(jb * 8 + i + 1) * 128], identity=identb[:])
                            return ins
                        sc.op("pe", tr, reads=[("xst", xi), "identb"], writes=[PSK[bank]])
                        dst = xgT[:, jb * 8:(jb + 1) * 8, tt * 128:(tt + 1) * 128]
                        if jb == 0:
                            sc.op("act", lambda e, dst=dst, bank=bank: e.activation(out=dst, in_=ps[bank][:].bitcast(BF16).rearrange("p (a b) -> p a b", a=8), func=AF.Copy),
                                  reads=[PSK[bank]], writes=["xgT"])
                        else:
                            sc.op("dve", lambda e, dst=dst, bank=bank: e.tensor_copy(out=dst, in_=ps[bank][:].bitcast(BF16).rearrange("p (a b) -> p a b", a=8)),
                                  reads=[PSK[bank]], writes=["xgT"])
                for fg in range(FF // 512):
                    wgb, wgk = load_w(wg[el, :, fg * 512:(fg + 1) * 512].rearrange("(k p) c -> p k c", p=128))
                    wub, wuk = load_w(wu[el, :, fg * 512:(fg + 1) * 512].rearrange("(k p) c -> p k c", p=128))
                    for fi in range(4):
                        kf = fg * 4 + fi
                        for tb in range(HT // 512):
                            par = st["n"] % 2
                            st["n"] += 1
                            gb, ub = 2 + par, 4 + par

                            def fgate(e, wgb=wgb, fi=fi, tb=tb, gb=gb):
                                for k in range(KD):
                                    ins = e.matmul(ps[gb][:], lhsT=wgb[:, k, fi * 128:(fi + 1) * 128], rhs=xgT[:, k, tb * 512:(tb + 1) * 512], start=(k == 0), stop=(k == KD - 1))
                                return ins

                            def fup(e, wub=wub, fi=fi, tb=tb, ub=ub):
                                for k in range(KD):
                                    ins = e.matmul(ps[ub][:], lhsT=wub[:, k, fi * 128:(fi + 1) * 128], rhs=xgT[:, k, tb * 512:(tb + 1) * 512], start=(k == 0), stop=(k == KD - 1))
                                return ins
                            sc.op("pe", fgate, reads=[wgk, "xgT"], writes=[PSK[gb]])
                            sc.op("pe", fup, reads=[wuk, "xgT"], writes=[PSK[ub]])
                            ti = st["tmp"] % NTMP
                            st["tmp"] += 1
                            sc.op("act", lambda e, ti=ti, gb=gb: e.activation(out=tmp[ti][:], in_=ps[gb][:], func=AF.Silu), reads=[PSK[gb]], writes=[("tmp", ti)])
                            sc.op("dve", lambda e, ti=ti, ub=ub, kf=kf, tb=tb: e.tensor_tensor(out=hidT[:, kf, tb * 512:(tb + 1) * 512], in0=tmp[ti][:], in1=ps[ub][:], op=ALU.mult),
                                  reads=[("tmp", ti), PSK[ub]], writes=[("hid", kf)])
                HID = [("hid", kf) for kf in range(FF // 128)]
                for db in range(4):
                    wd0, wd0k = load_w(wd[el, 0:2048, db * 512:(db + 1) * 512].rearrange("(k p) c -> p k c", p=128))
                    wd1, wd1k = load_w(wd[el, 2048:4096, db * 512:(db + 1) * 512].rearrange("(k p) c -> p k c", p=128))
                    for tt in range(HT // 128):
                        bank = (6, 7, 0, 1)[st["n"] % 4]
                        st["n"] += 1

                        def fdown(e, wd0=wd0, wd1=wd1, tt=tt, bank=bank):
                            for k in range(32):
                                wsrc = wd0 if k < 16 else wd1
                                ins = e.matmul(ps[bank][:], lhsT=hidT[:, k, tt * 128:(tt + 1) * 128], rhs=wsrc[:, k % 16, :], start=(k == 0), stop=(k == 31))
                            return ins
                        sc.op("pe", fdown, reads=[wd0k, wd1k] + HID, writes=[PSK[bank]])
                        oi = st["os"] % NOS
                        st["os"] += 1
                        if oi % 2 == 0:
                            sc.op("act", lambda e, oi=oi, bank=bank: e.activation(out=ostg[oi][:], in_=ps[bank][:], func=AF.Copy), reads=[PSK[bank]], writes=[("os", oi)])
                        else:
                            sc.op("dve", lambda e, oi=oi, bank=bank: e.tensor_copy(out=ostg[oi][:], in_=ps[bank][:]), reads=[PSK[bank]], writes=[("os", oi)])
                        r0 = base + tt * 128
                        sc.op("sp", lambda e, oi=oi, r0=r0, db=db: e.dma_start(out=o[r0:r0 + 128, db * 512:(db + 1) * 512], in_=ostg[oi][:]),
                              reads=[("os", oi)], dma="ost%d" % oi)
        sc.final_wait("sp", ["ost%d" % i for i in range(NOS)])
        sc.emit()
    return nc


def build_p3():
    nc = bass.Bass("TRN2", target_bir_lowering=False)
    with ExitStack() as es:
        y_in = nc.dram_tensor("y_in", [S, D], F32, kind="ExternalInput").ap()
        og = nc.dram_tensor("og", [NE * CAP, D], F32, kind="ExternalInput").ap()
        idxT_d = nc.dram_tensor("idxT", [128, 2 * NE], U32, kind="ExternalInput").ap()
        gateT_d = nc.dram_tensor("gateT", [128, 2 * NE], F32, kind="ExternalInput").ap()
        ln2g_d = nc.dram_tensor("ln2g", [128, D], F32, kind="ExternalInput").ap()
        ln2b_d = nc.dram_tensor("ln2b", [128, D], F32, kind="ExternalInput").ap()
        out = nc.dram_tensor("out", [S, D], F32, kind="ExternalOutput").ap()
        YW = nc.dram_tensor("YW", [S, D], F32).ap()
        sc = Sched(nc, es)
        sb = lambda name, shape, dt=F32: es.enter_context(nc.sbuf_tensor("s_" + name, list(shape), dt))
        idxT = sb("idxT", [128, 2 * NE], U32)
        gateT = sb("gateT", [128, 2 * NE])
        lng = sb("lng", [128, D])
        lnb = sb("lnb", [128, D])
        smalls = sb("smalls", [128, 8])
        stats = sb("stats", [128, 4, 6])
        mv = sb("mv", [128, 8])
        ot = [sb("ot%d" % i, [128, D]) for i in range(3)]
        zt = [sb("z%d" % i, [128, D]) for i in range(2)]
        x1f = sb("x1f", [128, D])
        for dst, src, key in ((idxT, idxT_d, "idxT"), (gateT, gateT_d, "gateT"), (lng, ln2g_d, "lng"), (lnb, ln2b_d, "lnb")):
            sc.op("sp", lambda e, dst=dst, src=src: e.dma_start(out=dst[:], in_=src), writes=[key], dma="c_" + key)
        sc.op("pool", lambda e: e.memset(smalls[:, 6:7], LN_EPS), writes=["eps_ln"])
        sc.op("pool", lambda e: e.dma_start(out=YW, in_=y_in), writes=["YW"], dma="yw")
        for j in range(2 * NE):
            i = j % 3
            sc.op("sp", lambda e, i=i, j=j: e.dma_start(out=ot[i][:], in_=og[j * 128:(j + 1) * 128, :]), writes=[("ot", i)], dma="ot%d" % i)
            sc.op("dve", lambda e, i=i, j=j: e.tensor_scalar(out=ot[i][:], in0=ot[i][:], scalar1=gateT[:, j:j + 1], scalar2=None, op0=ALU.mult),
                  reads=[("ot", i), "gateT"], writes=[("ot", i)])
            sc.op("pool", lambda e, i=i, j=j: e.indirect_dma_start(
                out=YW, out_offset=bass.IndirectOffsetOnAxis(ap=idxT[:, j:j + 1], axis=0), in_=ot[i][:], in_offset=None, compute_op=ALU.add),
                reads=[("ot", i), "idxT"], writes=["YW"], dma="yw")
        for t in range(NT):
            zi = t % 2
            sc.op("sp", lambda e, zi=zi, t=t: e.dma_start(out=zt[zi][:], in_=YW[t * 128:(t + 1) * 128, :]), reads=["YW"], writes=[("z", zi)], dma="yld%d" % zi)
            _ln_tile(sc, zt[zi][:], ("z", zi), x1f[:], "x1f", stats, mv, smalls, lng, lnb)
            sc.op("sp", lambda e, t=t: e.dma_start(out=out[t * 128:(t + 1) * 128, :], in_=x1f[:]), reads=["x1f"], dma="out_st")
        sc.final_wait("sp", ["out_st"])
        sc.emit()
    return nc


_NC_CACHE = {}


def kernel(x, w_in, b_gate, a_q_norm, a_k_norm, b_lambda, b_subln, w_a_proj, w_b_proj,
           w_o, ln1_g, ln1_b, w_router, w_gate, w_up, w_down, ln2_g, ln2_b):
    f32 = np.float32
    A = lambda a: np.ascontiguousarray(np.asarray(a, dtype=f32))
    x = A(x)
    for name, fn in (("p1", build_p1), ("p2", build_p2), ("p3", build_p3)):
        if name not in _NC_CACHE:
            _NC_CACHE[name] = fn()
    cA, sA, cB, sB, permA, permB = _rope_tables()
    rep = lambda v: np.ascontiguousarray(np.broadcast_to(A(v).reshape(1, -1), (128, A(v).size)))
    identb = np.eye(128, dtype=f32).astype(ml_dtypes.bfloat16)
    shared = {
        "w_in": A(w_in[0]),
        "bgT": np.ascontiguousarray(A(b_gate[0]).reshape(32, 128).T),
        "gq": A(a_q_norm[0]).reshape(128, 1),
        "gk": A(a_k_norm[0]).reshape(128, 1),
        "lamb": rep(b_lambda[0]),
        "subln": np.ascontiguousarray(A(b_subln[0]).reshape(2, 128).T),
        "w_a": A(w_a_proj[0]), "w_b": A(w_b_proj[0]), "w_o": A(w_o[0]),
        "ln1g": rep(ln1_g[0]), "ln1b": rep(ln1_b[0]),
        "w_r": A(w_router[0]),
        "ropeA_c": cA, "ropeA_s": sA, "ropeB_c": cB, "ropeB_s": sB,
        "permA": permA, "permB": permB,
        "identf": np.eye(128, dtype=f32), "identb": identb,
    }
    cores = list(range(NCORE))
    r1 = run_bass_kernel_spmd(_NC_CACHE["p1"], [dict(shared, x=x[c]) for c in cores], core_ids=cores).results
    xg_all = np.stack([np.asarray(r["xg_out"]) for r in r1], 0).reshape(NCORE, NCORE, 2, CAP, D)
    xg_p2 = np.ascontiguousarray(xg_all.transpose(1, 2, 0, 3, 4)).reshape(NCORE, 2 * NCORE * CAP, D)
    del xg_all
    wg_all, wu_all, wd_all = A(w_gate[0]), A(w_up[0]), A(w_down[0])
    r2 = run_bass_kernel_spmd(_NC_CACHE["p2"], [
        {"xg": xg_p2[c], "wg": wg_all[2 * c:2 * c + 2], "wu": wu_all[2 * c:2 * c + 2], "wd": wd_all[2 * c:2 * c + 2], "identb": identb}
        for c in cores], core_ids=cores).results
    o_all = np.stack([np.asarray(r["o"], dtype=f32) for r in r2], 0).reshape(NCORE, 2, NCORE, CAP, D)
    og_p3 = np.ascontiguousarray(o_all.transpose(2, 0, 1, 3, 4)).reshape(NCORE, NE * CAP, D)
    del o_all
    lay = lambda a: np.ascontiguousarray(np.asarray(a).reshape(NE, 2, 128).transpose(2, 0, 1).reshape(128, 2 * NE))
    ln2g, ln2b = rep(ln2_g[0]), rep(ln2_b[0])
    r3 = run_bass_kernel_spmd(_NC_CACHE["p3"], [
        {"y_in": np.asarray(r1[c]["y_out"], dtype=f32), "og": og_p3[c],
         "idxT": lay(np.asarray(r1[c]["idx_out"]).astype(np.uint32)), "gateT": lay(np.asarray(r1[c]["gate_out"], dtype=f32)),
         "ln2g": ln2g, "ln2b": ln2b} for c in cores], core_ids=cores).results
    return np.stack([np.asarray(r["out"], dtype=f32) for r in r3], axis=0)
```

```python
import math
from contextlib import ExitStack

import numpy as np
import ml_dtypes

import concourse.bass as bass
import concourse.mybir as mybir
from concourse.bass_utils import run_bass_kernel_spmd

F32 = mybir.dt.float32
BF16 = mybir.dt.bfloat16
U32 = mybir.dt.uint32
AF = mybir.ActivationFunctionType
ALU = mybir.AluOpType
AX = mybir.AxisListType

S = 2048
D = 2048
NT = 16
KD = 16
NCORE = 8
NE = 16
CAP = 256
FF = 4096
IN_COLS = 8704
ALPHA = 2.0 ** 0.25
LAM_INIT = 0.2
RMS_EPS = 1e-6
LN_EPS = 1e-5
SCALE = 128.0 ** -0.5


class Sched:
    ENG = ("pe", "act", "dve", "pool", "sp")

    def __init__(self, nc, es):
        self.nc = nc
        self.es = es
        self.prog = {e: [] for e in self.ENG}
        self.semh = {}
        self.cnt = {}
        for e in self.ENG:
            self.semh[e] = es.enter_context(nc.semaphore("sem_" + e))
            self.cnt[e] = 0
        self.waited = {e: {} for e in self.ENG}
        self.lastw = {}
        self.readers = {}

    def _slot(self, name):
        if name not in self.semh:
            self.semh[name] = self.es.enter_context(self.nc.semaphore("d_" + name))
            self.cnt[name] = 0
        return name

    def op(self, eng, fn, reads=(), writes=(), dma=None):
        deps = {}

        def add(tok):
            if tok is None:
                return
            name, val = tok
            if name == "pe" and eng == "pe":
                return
            if deps.get(name, 0) < val:
                deps[name] = val

        for k in reads:
            add(self.lastw.get(k))
        for k in writes:
            add(self.lastw.get(k))
            for t in self.readers.get(k, {}).items():
                add(t)
        for name, val in deps.items():
            if self.waited[eng].get(name, 0) >= val:
                continue
            self.waited[eng][name] = val
            sem = self.semh[name]
            self.prog[eng].append(lambda e, sem=sem, val=val: e.wait_ge(sem, val))
        if dma is not None:
            name = self._slot(dma)
            self.cnt[name] += 16
            inc = 16
        else:
            name = eng
            self.cnt[name] += 1
            inc = 1
        tok = (name, self.cnt[name])
        sem = self.semh[name]
        self.prog[eng].append(lambda e, fn=fn, sem=sem, inc=inc: fn(e).then_inc(sem, inc))
        for k in reads:
            self.readers.setdefault(k, {})[name] = tok[1]
        for k in writes:
            self.lastw[k] = tok
            self.readers[k] = {}
        return tok

    def barrier(self):
        for eng in self.ENG:
            for name, sem in self.semh.items():
                val = self.cnt[name]
                if val == 0 or self.waited[eng].get(name, 0) >= val:
                    continue
                self.waited[eng][name] = val
                self.prog[eng].append(lambda e, sem=sem, val=val: e.wait_ge(sem, val))

    def final_wait(self, eng, names):
        for name in names:
            val = self.cnt[name]
            sem = self.semh[name]
            self.prog[eng].append(lambda e, sem=sem, val=val: e.wait_ge(sem, val))

    def emit(self):
        nc = self.nc
        with nc.Block() as block:
            @block.tensor
            def _(e):
                for f in self.prog["pe"]:
                    f(e)

            @block.scalar
            def _(e):
                for f in self.prog["act"]:
                    f(e)

            @block.vector
            def _(e):
                for f in self.prog["dve"]:
                    f(e)

            @block.gpsimd
            def _(e):
                for f in self.prog["pool"]:
                    f(e)

            @block.sync
            def _(e):
                for f in self.prog["sp"]:
                    f(e)


def build_p1(debug=False):
    nc = bass.Bass("TRN2", target_bir_lowering=False)
    es = ExitStack()
    with es:
        _build(nc, es, debug)
    return nc


def _build(nc, es, debug):
    def din(name, shape, dt=F32):
        return nc.dram_tensor(name, list(shape), dt, kind="ExternalInput").ap()

    def dint(name, shape, dt):
        return nc.dram_tensor(name, list(shape), dt, kind="Internal").ap()

    x = din("x", [S, D])
    w_in = din("w_in", [D, IN_COLS])
    bgT = din("bgT", [128, 32])
    gq_d = din("gq", [128, 1])
    gk_d = din("gk", [128, 1])
    lamb_d = din("lamb", [128, 512])
    subln_d = din("subln", [128, 2])
    w_a = din("w_a", [1024, D])
    w_b = din("w_b", [1024, D])
    w_o = din("w_o", [D, D])
    ln1g_d = din("ln1g", [128, D])
    ln1b_d = din("ln1b", [128, D])
    w_r = din("w_r", [D, NE])
    ropeA_c = din("ropeA_c", [128, S])
    ropeA_s = din("ropeA_s", [128, S])
    ropeB_c = din("ropeB_c", [128, S])
    ropeB_s = din("ropeB_s", [128, S])
    permA_d = din("permA", [128, 128])
    permB_d = din("permB", [128, 128])
    identf_d = din("identf", [128, 128])
    identb_d = din("identb", [128, 128], BF16)
    Y = nc.dram_tensor("y_out", [S, D], F32, kind="ExternalOutput").ap()
    xg_out = nc.dram_tensor("xg_out", [NE * CAP, D], BF16, kind="ExternalOutput").ap()
    idx_out = nc.dram_tensor("idx_out", [NE, CAP], U32, kind="ExternalOutput").ap()
    gate_out = nc.dram_tensor("gate_out", [NE, CAP], F32, kind="ExternalOutput").ap()
    if debug:
        dbg = nc.dram_tensor("dbg", [S, D], F32, kind="ExternalOutput").ap()
        dbg_ot = nc.dram_tensor("dbg_ot", [16 * 128, S], BF16, kind="ExternalOutput").ap()
        dbg_g = nc.dram_tensor("dbg_g", [32 * 128, S], F32, kind="ExternalOutput").ap()
        dbg_z = nc.dram_tensor("dbg_z", [S, D], F32, kind="ExternalOutput").ap()

    OT = dint("OT", [16 * 128, S], BF16)
    GATES = dint("GATES", [32 * 128, S], F32)
    X1B = dint("X1B", [S, D], BF16)
    LOGT = dint("LOGT", [NE, S], F32)

    sc = Sched(nc, es)
    scope = [es]
    sb = lambda name, shape, dt=F32: scope[0].enter_context(nc.sbuf_tensor("s_" + name, list(shape), dt))
    ps = [es.enter_context(nc.psum_tensor("ps%d" % i, [128, 512], F32)) for i in range(8)]
    PSK = [("ps", i) for i in range(8)]

    identf = sb("identf", [128, 128])
    identb = sb("identb", [128, 128], BF16)
    permA = sb("permA", [128, 128])
    permB = sb("permB", [128, 128])
    onesf = sb("onesf", [128, 128])
    onesb = sb("onesb", [128, 128], BF16)
    bg = sb("bg", [128, 32])
    gq = sb("gqs", [128, 1])
    gk = sb("gks", [128, 1])
    lamb = sb("lambs", [128, 512])
    subg = sb("subg", [128, 2])
    neglam = sb("neglam", [128, 1])
    smalls = sb("smalls", [128, 8])
    wr_sb = sb("wr_sb", [128, KD, NE])

    def ld(dst, src, key, q="sp"):
        sc.op(q, lambda e: e.dma_start(out=dst, in_=src), writes=[key], dma="c_" + key)

    ld(identf[:], identf_d, "identf")
    ld(identb[:], identb_d, "identb")
    ld(permA[:], permA_d, "permA")
    ld(permB[:], permB_d, "permB")
    ld(bg[:], bgT, "bg")
    ld(gq[:], gq_d, "gq")
    ld(gk[:], gk_d, "gk")
    ld(lamb[:], lamb_d, "lamb")
    ld(subg[:], subln_d, "subg")
    ld(wr_sb[:], w_r.rearrange("(k p) e -> p k e", p=128), "wr")
    sc.op("pool", lambda e: e.memset(onesf[:], 1.0), writes=["onesf"])
    sc.op("pool", lambda e: e.memset(onesb[:], 1.0), writes=["onesb"])
    lt = sb("lam_t", [128, 256])
    sc.op("dve", lambda e: e.tensor_tensor(out=lt[:, 0:128], in0=lamb[:, 0:128], in1=lamb[:, 128:256], op=ALU.mult),
          reads=["lamb"], writes=["lt"])
    sc.op("dve", lambda e: e.tensor_tensor(out=lt[:, 128:256], in0=lamb[:, 256:384], in1=lamb[:, 384:512], op=ALU.mult),
          reads=["lamb"], writes=["lt"])
    sc.op("dve", lambda e: e.reduce_sum(out=smalls[:, 0:1], in_=lt[:, 0:128], axis=AX.X), reads=["lt"], writes=["sm0"])
    sc.op("dve", lambda e: e.reduce_sum(out=smalls[:, 1:2], in_=lt[:, 128:256], axis=AX.X), reads=["lt"], writes=["sm1"])
    sc.op("act", lambda e: e.activation(out=smalls[:, 2:4], in_=smalls[:, 0:2], func=AF.Exp), reads=["sm0", "sm1"], writes=["sm2"])
    sc.op("dve", lambda e: e.tensor_tensor(out=smalls[:, 4:5], in0=smalls[:, 3:4], in1=smalls[:, 2:3], op=ALU.subtract),
          reads=["sm2"], writes=["sm4"])
    sc.op("dve", lambda e: e.tensor_scalar(out=neglam[:], in0=smalls[:, 4:5], scalar1=-LAM_INIT, scalar2=None, op0=ALU.add),
          reads=["sm4"], writes=["neglam"])
    sc.op("dve", lambda e: e.tensor_scalar(out=subg[:], in0=subg[:], scalar1=1.0 - LAM_INIT, scalar2=None, op0=ALU.mult),
          reads=["subg"], writes=["subg"])

    xT = sb("xT", [128, KD, S], BF16)
    NW = 3
    wbuf = [sb("wbuf%d" % i, [128, 16, 512], BF16) for i in range(NW)]
    xs = [sb("xs%d" % i, [128, D]) for i in range(2)]
    NTMP = 8
    tmp = [sb("tmp%d" % i, [128, 512]) for i in range(NTMP)]
    es_p1 = ExitStack()
    scope[0] = es_p1
    Qb = sb("Qb", [128, 4, S], BF16)
    Kb = sb("Kb", [128, 2, S], BF16)
    Vb = sb("Vb", [128, NT, 256], BF16)
    NPB = 4
    pT = [sb("pT%d" % i, [128, 512], BF16) for i in range(NPB)]
    ost = [sb("ost%d" % i, [128, 512], BF16) for i in range(2)]
    o0 = [sb("o0_%d" % i, [128, 512]) for i in range(2)]
    o1 = [sb("o1_%d" % i, [128, 512]) for i in range(2)]
    rc = [sb("rc%d" % i, [128, 512]) for i in range(2)]
    rs_ = [sb("rs%d" % i, [128, 512]) for i in range(2)]

    state = {"w": 0, "tmp": 0, "pT": 0, "ost": 0}

    def load_w(src3, nk, ncols, pieces=None):
        i = state["w"] % NW
        state["w"] += 1
        key = ("w", i)
        if pieces is None:
            pieces = [(0, src3)]
        for (k0, srcp) in pieces:
            nkp = srcp.shape[1]
            sc.op("pool", lambda e, srcp=srcp, k0=k0, nkp=nkp: e.dma_start(out=wbuf[i][:, k0:k0 + nkp, 0:ncols], in_=srcp),
                  writes=[key], dma="w%d" % i)
        return wbuf[i], key

    def get_tmp():
        i = state["tmp"] % NTMP
        state["tmp"] += 1
        return tmp[i], ("tmp", i)

    def wslab(w2d, c0, ncols):
        return w2d[:, c0:c0 + ncols].rearrange("(k p) c -> p k c", p=128)

    for t in range(NT):
        xst = xs[t % 2]
        xk = ("xs", t % 2)
        sc.op("sp", lambda e, xst=xst, t=t: e.dma_start(out=xst[:], in_=x[t * 128:(t + 1) * 128, :]),
              writes=[xk], dma="xs%d" % (t % 2))
        for j in range(4):
            bank = (t * 4 + j) % 4

            def tr(e, xst=xst, j=j, bank=bank):
                for i in range(4):
                    ins = e.transpose(out=ps[bank][:, i * 128:(i + 1) * 128],
                                      in_=xst[:, (4 * j + i) * 128:(4 * j + i + 1) * 128], identity=identf[:])
                return ins
            sc.op("pe", tr, reads=[xk, "identf"], writes=[PSK[bank]])
            eng = "act" if j % 2 == 0 else "dve"
            dst = xT[:, 4 * j:4 * j + 4, t * 128:(t + 1) * 128]
            srcv = ps[bank][:].rearrange("p (a b) -> p a b", a=4)
            if eng == "act":
                sc.op("act", lambda e, dst=dst, srcv=srcv: e.activation(out=dst, in_=srcv, func=AF.Copy),
                      reads=[PSK[bank]], writes=[("xT", t)])
            else:
                sc.op("dve", lambda e, dst=dst, srcv=srcv: e.tensor_copy(out=dst, in_=srcv),
                      reads=[PSK[bank]], writes=[("xT", t)])
    XT_ALL = [("xT", t) for t in range(NT)]

    def inproj_fm(wb, wkey, coff, blk, bank):
        def f(e):
            for k in range(KD):
                ins = e.matmul(ps[bank][:], lhsT=wb[:, k, coff:coff + 128], rhs=xT[:, k, blk * 512:(blk + 1) * 512],
                               start=(k == 0), stop=(k == KD - 1))
            return ins
        sc.op("pe", f, reads=[wkey] + [("xT", t) for t in range(blk * 4, blk * 4 + 4)], writes=[PSK[bank]])

    def load_rope(blk, which):
        i = blk % 2
        c_d, s_d = (ropeA_c, ropeA_s) if which == "A" else (ropeB_c, ropeB_s)
        sc.op("sp", lambda e: e.dma_start(out=rc[i][:], in_=c_d[:, blk * 512:(blk + 1) * 512]), writes=[("rc", i)], dma="rc%d" % i)
        sc.op("sp", lambda e: e.dma_start(out=rs_[i][:], in_=s_d[:, blk * 512:(blk + 1) * 512]), writes=[("rs", i)], dma="rs%d" % i)
        return i

    pending = []

    def flush(keep):
        while len(pending) > keep:
            pending.pop(0)()

    def rope_chunk(bank, bank2, bank3, ri, dst, dkey, gain, perm, permkey, rms):
        qg, qgk = get_tmp()
        if gain is not None:
            sc.op("act", lambda e: e.activation(out=qg[:], in_=ps[bank][:], func=AF.Copy, scale=gain[:]),
                  reads=[PSK[bank], "gq", "gk"], writes=[qgk])
        else:
            sc.op("act", lambda e: e.activation(out=qg[:], in_=ps[bank][:], func=AF.Copy),
                  reads=[PSK[bank]], writes=[qgk])
        sq = sqk = None
        if rms:
            sq, sqk = get_tmp()
            sc.op("act", lambda e: e.activation(out=sq[:], in_=ps[bank][:], func=AF.Square),
                  reads=[PSK[bank]], writes=[sqk])
        t1, t1k = get_tmp()
        sc.op("pool", lambda e: e.tensor_tensor(out=t1[:], in0=qg[:], in1=rc[ri][:], op=ALU.mult),
              reads=[qgk, ("rc", ri)], writes=[t1k])
        t2, t2k = get_tmp()

        def part2():
            if rms:
                sc.op("pe", lambda e: e.matmul(ps[bank2][:], lhsT=onesf[:], rhs=sq[:], start=True, stop=True),
                      reads=[sqk, "onesf"], writes=[PSK[bank2]])
                sc.op("act", lambda e: e.activation(out=sq[:], in_=ps[bank2][:], func=AF.Ln, scale=1.0 / 128.0, bias=smalls[:, 5:6]),
                      reads=[PSK[bank2], "eps_rms"], writes=[sqk])
                sc.op("act", lambda e: e.activation(out=sq[:], in_=sq[:], func=AF.Exp, scale=-0.5),
                      reads=[sqk], writes=[sqk])
            sc.op("pe", lambda e: e.matmul(ps[bank3][:], lhsT=perm[:], rhs=qg[:], start=True, stop=True),
                  reads=[qgk, permkey], writes=[PSK[bank3]])
            sc.op("dve", lambda e: e.tensor_tensor(out=t2[:], in0=ps[bank3][:], in1=rs_[ri][:], op=ALU.mult),
                  reads=[PSK[bank3], ("rs", ri)], writes=[t2k])
            if rms:
                sc.op("dve", lambda e: e.tensor_tensor(out=t1[:], in0=t1[:], in1=t2[:], op=ALU.add),
                      reads=[t1k, t2k], writes=[t1k])
                sc.op("dve", lambda e: e.tensor_tensor(out=dst, in0=t1[:], in1=sq[:], op=ALU.mult),
                      reads=[t1k, sqk], writes=[dkey])
            else:
                sc.op("dve", lambda e: e.tensor_tensor(out=dst, in0=t1[:], in1=t2[:], op=ALU.add),
                      reads=[t1k, t2k], writes=[dkey])
        pending.append(part2)
        flush(1)

    rb_no = [0]

    def rbanks():
        rb_no[0] += 1
        return (0, 1, 2) if rb_no[0] % 2 else (3, 6, 7)

    sc.op("pool", lambda e: e.memset(smalls[:, 5:6], RMS_EPS), writes=["eps_rms"])
    sc.op("pool", lambda e: e.memset(smalls[:, 6:7], LN_EPS), writes=["eps_ln"])

    def inproj_v(wb, wkey, dv):
        per = 512 // dv
        for t0 in range(0, NT, per):
            bank = 4 + (t0 // per) % 2

            def f(e, t0=t0, bank=bank):
                for i in range(per):
                    t = t0 + i
                    for k in range(KD):
                        ins = e.matmul(ps[bank][:, i * dv:(i + 1) * dv], lhsT=xT[:, k, t * 128:(t + 1) * 128],
                                       rhs=wb[:, k, 0:dv], start=(k == 0), stop=(k == KD - 1))
                return ins
            sc.op("pe", f, reads=[wkey] + XT_ALL[t0:t0 + per], writes=[PSK[bank]])
            srcv = ps[bank][:].rearrange("p (a b) -> p a b", a=per)
            sc.op("act", lambda e, t0=t0, srcv=srcv: e.activation(out=Vb[:, t0:t0 + per, 0:dv], in_=srcv, func=AF.Copy),
                  reads=[PSK[bank]], writes=["V"])

    unit_no = [0]

    def attention_unit(qi, ki, blk, dv, epilogue):
        u = unit_no[0] % 2
        unit_no[0] += 1
        nh = dv // 128
        obank = [2 + 2 * u, 3 + 2 * u]
        lbank = 6 + u
        qap = Qb[:, qi, blk * 512:(blk + 1) * 512]
        pk = [None] * NT

        def qk(kt):
            sb_ = kt % 2
            sc.op("pe", lambda e: e.matmul(ps[sb_][:], lhsT=Kb[:, ki, kt * 128:(kt + 1) * 128], rhs=qap, start=True, stop=True),
                  reads=[("Q", qi), ("K", ki)], writes=[PSK[sb_]])
            i = state["pT"] % NPB
            state["pT"] += 1
            pk[kt] = i
            sc.op("act", lambda e: e.activation(out=pT[i][:], in_=ps[sb_][:], func=AF.Exp, scale=SCALE),
                  reads=[PSK[sb_]], writes=[("pT", i)])

        def pv(kt):
            i = pk[kt]

            def f(e):
                for h in range(nh):
                    e.matmul(ps[obank[h]][:], lhsT=Vb[:, kt, h * 128:(h + 1) * 128], rhs=pT[i][:],
                             start=(kt == 0), stop=(kt == NT - 1))
                return e.matmul(ps[lbank][:], lhsT=onesb[:], rhs=pT[i][:], start=(kt == 0), stop=(kt == NT - 1))
            sc.op("pe", f, reads=[("pT", i), "V", "onesb"], writes=[PSK[obank[h]] for h in range(nh)] + [PSK[lbank]])

        qk(0)
        qk(1)
        for kt in range(NT):
            pv(kt)
            if kt + 2 < NT:
                qk(kt + 2)
        rl, rlk = get_tmp()
        sc.op("dve", lambda e: e.reciprocal(out=rl[:], in_=ps[lbank][:]), reads=[PSK[lbank]], writes=[rlk])
        epilogue(obank, rl, rlk)

    def store_oT(src_tile, skey, chunk, blk):
        sc.op("sp", lambda e: e.dma_start(out=OT[chunk * 128:(chunk + 1) * 128, blk * 512:(blk + 1) * 512], in_=src_tile[:]),
              reads=[skey], dma="ost_%s" % str(skey))

    for hk in range(2):
        wq, wqk = load_w(wslab(w_in, hk * 512, 512), KD, 512)
        wk_, wkk = load_w(wslab(w_in, 1024 + hk * 128, 128), KD, 128)
        wv_, wvk = load_w(wslab(w_in, 1280 + hk * 128, 128), KD, 128)
        for blk in range(4):
            ri = load_rope(blk, "A")
            for g in range(4):
                b0, b1, b2 = rbanks()
                inproj_fm(wq, wqk, g * 128, blk, b0)
                rope_chunk(b0, b1, b2, ri, Qb[:, g, blk * 512:(blk + 1) * 512], ("Q", g), gq, permA, "permA", True)
            b0, b1, b2 = rbanks()
            inproj_fm(wk_, wkk, 0, blk, b0)
            rope_chunk(b0, b1, b2, ri, Kb[:, 0, blk * 512:(blk + 1) * 512], ("K", 0), gk, permA, "permA", True)
        inproj_v(wv_, wvk, 128)
        flush(0)
        for g in range(4):
            for blk in range(4):
                def epiA(obank, rl, rlk, g=g, blk=blk):
                    i = state["ost"] % 2
                    state["ost"] += 1
                    sc.op("dve", lambda e: e.tensor_tensor(out=ost[i][:], in0=ps[obank[0]][:], in1=rl[:], op=ALU.mult),
                          reads=[PSK[obank[0]], rlk], writes=[("ost", i)])
                    store_oT(ost[i], ("ost", i), hk * 4 + g, blk)
                attention_unit(g, 0, blk, 128, epiA)

    for h in range(4):
        wq, wqk = load_w(wslab(w_in, 1536 + h * 256, 256), KD, 256)
        wk_, wkk = load_w(wslab(w_in, 2560 + h * 256, 256), KD, 256)
        wv_, wvk = load_w(wslab(w_in, 3584 + h * 256, 256), KD, 256)
        for blk in range(4):
            ri = load_rope(blk, "B")
            for c in range(2):
                b0, b1, b2 = rbanks()
                inproj_fm(wq, wqk, c * 128, blk, b0)
                rope_chunk(b0, b1, b2, ri, Qb[:, c, blk * 512:(blk + 1) * 512], ("Q", c), None, permB, "permB", False)
                b0, b1, b2 = rbanks()
                inproj_fm(wk_, wkk, c * 128, blk, b0)
                rope_chunk(b0, b1, b2, ri, Kb[:, c, blk * 512:(blk + 1) * 512], ("K", c), None, permB, "permB", False)
        inproj_v(wv_, wvk, 256)
        flush(0)
        for blk in range(4):
            def epiB0(obank, rl, rlk):
                for hf in range(2):
                    sc.op("dve", lambda e, hf=hf: e.tensor_tensor(out=o0[hf][:], in0=ps[obank[hf]][:], in1=rl[:], op=ALU.mult),
                          reads=[PSK[obank[hf]], rlk], writes=[("o0", hf)])

            def epiB1(obank, rl, rlk, h=h, blk=blk):
                sqs = []
                for hf in range(2):
                    sc.op("dve", lambda e, hf=hf: e.tensor_tensor(out=o1[hf][:], in0=ps[obank[hf]][:], in1=rl[:], op=ALU.mult),
                          reads=[PSK[obank[hf]], rlk], writes=[("o1", hf)])
                    sc.op("dve", lambda e, hf=hf: e.scalar_tensor_tensor(out=o1[hf][:], in0=o1[hf][:], scalar=neglam[:], in1=o0[hf][:],
                                                                      op0=ALU.mult, op1=ALU.add),
                          reads=[("o1", hf), ("o0", hf), "neglam"], writes=[("o1", hf)])
                    sq, sqk = get_tmp()
                    sc.op("act", lambda e, hf=hf, sq=sq: e.activation(out=sq[:], in_=o1[hf][:], func=AF.Square),
                          reads=[("o1", hf)], writes=[sqk])
                    sqs.append((sq, sqk))
                lb = obank[0]

                def f(e):
                    e.matmul(ps[lb][:], lhsT=onesf[:], rhs=sqs[0][0][:], start=True, stop=False)
                    return e.matmul(ps[lb][:], lhsT=onesf[:], rhs=sqs[1][0][:], start=False, stop=True)
                sc.op("pe", f, reads=[sqs[0][1], sqs[1][1], "onesf"], writes=[PSK[lb]])
                rstd, rk = get_tmp()
                sc.op("act", lambda e: e.activation(out=rstd[:], in_=ps[lb][:], func=AF.Ln, scale=1.0 / 256.0, bias=smalls[:, 5:6]),
                      reads=[PSK[lb], "eps_rms"], writes=[rk])
                sc.op("act", lambda e: e.activation(out=rstd[:], in_=rstd[:], func=AF.Exp, scale=-0.5), reads=[rk], writes=[rk])
                for hf in range(2):
                    i = state["ost"] % 2
                    state["ost"] += 1
                    sc.op("dve", lambda e, hf=hf, i=i: e.scalar_tensor_tensor(out=ost[i][:], in0=o1[hf][:], scalar=subg[:, hf:hf + 1], in1=rstd[:],
                                                                           op0=ALU.mult, op1=ALU.mult),
                          reads=[("o1", hf), "subg", rk], writes=[("ost", i)])
                    store_oT(ost[i], ("ost", i), 8 + h * 2 + hf, blk)
            attention_unit(0, 0, blk, 256, epiB0)
            attention_unit(1, 1, blk, 256, epiB1)

    for cg in range(8):
        wgt, wgk = load_w(wslab(w_in, 4608 + cg * 512, 512), KD, 512)
        for jj in range(4):
            j = cg * 4 + jj
            for blk in range(4):
                bank = (jj * 4 + blk) % 4
                inproj_fm(wgt, wgk, jj * 128, blk, bank)
                tt_, tk = get_tmp()
                sc.op("act", lambda e, tt_=tt_, bank=bank, j=j: e.activation(out=tt_[:], in_=ps[bank][:], func=AF.Sigmoid, bias=bg[:, j:j + 1]),
                      reads=[PSK[bank], "bg"], writes=[tk])
                sc.op("sp", lambda e, tt_=tt_, j=j, blk=blk: e.dma_start(out=GATES[j * 128:(j + 1) * 128, blk * 512:(blk + 1) * 512], in_=tt_[:]),
                      reads=[tk], writes=[("GATES", j, blk)], dma="gst_%s" % str(tk))

    sc.barrier()
    es_p1.close()
    if debug:
        sc.op("sp", lambda e: e.dma_start(out=dbg_ot, in_=OT), dma="dbg_st")
        sc.op("sp", lambda e: e.dma_start(out=dbg_g, in_=GATES), dma="dbg_st")
    es_p2 = es.enter_context(ExitStack())
    scope[0] = es_p2
    oTg = [xT[:, 0:16, 0:512], xT[:, 0:16, 512:1024]]
    mrg = xT[:, 0:16, 1024:1536]
    zt = [sb("z%d" % i, [128, D]) for i in range(4)]
    x1f = xs[0]
    x1b = sb("x1b", [128, D], BF16)
    ya_t = xs[1]
    lng = sb("lng", [128, D])
    lnb = sb("lnb", [128, D])
    stats = sb("stats", [128, 4, 6])
    mv = sb("mv", [128, 8])
    ld(lng[:], ln1g_d, "lng")
    ld(lnb[:], ln1b_d, "lnb")

    for g in range(4):
        og = oTg[g % 2]
        ogk = ("oTg", g % 2)
        sc.op("sp", lambda e, og=og, g=g: e.dma_start(out=og, in_=OT[:, g * 512:(g + 1) * 512].rearrange("(k p) s -> p k s", p=128)),
              writes=[ogk], dma="oTg%d" % (g % 2))
        for jj in range(8):
            wab, wabk = load_w(None, 0, 256, pieces=[(0, w_a[:, jj * 256:(jj + 1) * 256].rearrange("(k p) c -> p k c", p=128)),
                                                     (8, w_b[:, jj * 256:(jj + 1) * 256].rearrange("(k p) c -> p k c", p=128))])
            for j2 in range(2):
                j = jj * 2 + j2

                def fa(e, j2=j2, og=og, wab=wab):
                    for k in range(8):
                        ins = e.matmul(ps[0][:], lhsT=wab[:, k, j2 * 128:(j2 + 1) * 128], rhs=og[:, k, :], start=(k == 0), stop=(k == 7))
                    return ins

                def fb(e, j2=j2, og=og, wab=wab):
                    for k in range(8):
                        ins = e.matmul(ps[1][:], lhsT=wab[:, 8 + k, j2 * 128:(j2 + 1) * 128], rhs=og[:, 8 + k, :], start=(k == 0), stop=(k == 7))
                    return ins
                sc.op("pe", fa, reads=[wabk, ogk], writes=[PSK[0]])
                sc.op("pe", fb, reads=[wabk, ogk], writes=[PSK[1]])
                g0, g0k = get_tmp()
                g1, g1k = get_tmp()
                sc.op("sp", lambda e, g0=g0, j=j, g=g: e.dma_start(out=g0[:], in_=GATES[j * 128:(j + 1) * 128, g * 512:(g + 1) * 512]),
                      reads=[("GATES", j, g)], writes=[g0k], dma="gld_%s" % str(g0k))
                sc.op("sp", lambda e, g1=g1, j=j, g=g: e.dma_start(out=g1[:], in_=GATES[(16 + j) * 128:(17 + j) * 128, g * 512:(g + 1) * 512]),
                      reads=[("GATES", 16 + j, g)], writes=[g1k], dma="gld_%s" % str(g1k))
                sc.op("dve", lambda e, g0=g0: e.tensor_tensor(out=g0[:], in0=ps[0][:], in1=g0[:], op=ALU.mult), reads=[PSK[0], g0k], writes=[g0k])
                sc.op("dve", lambda e, g1=g1: e.tensor_tensor(out=g1[:], in0=ps[1][:], in1=g1[:], op=ALU.mult), reads=[PSK[1], g1k], writes=[g1k])
                sc.op("dve", lambda e, g0=g0, g1=g1, j=j: e.tensor_tensor(out=mrg[:, j, :], in0=g0[:], in1=g1[:], op=ALU.add),
                      reads=[g0k, g1k], writes=[("mrg", j)])
        MRG = [("mrg", j) for j in range(16)]
        for eb in range(4):
            wob, wobk = load_w(wslab(w_o, eb * 512, 512), KD, 512)
            for tt in range(4):
                t = g * 4 + tt
                bank = 2 + (eb * 4 + tt) % 4

                def fo(e, tt=tt, bank=bank, wob=wob):
                    for k in range(KD):
                        ins = e.matmul(ps[bank][:], lhsT=mrg[:, k, tt * 128:(tt + 1) * 128], rhs=wob[:, k, :], start=(k == 0), stop=(k == KD - 1))
                    return ins
                sc.op("pe", fo, reads=[wobk] + MRG, writes=[PSK[bank]])
                xr, xrk = get_tmp()
                sc.op("sp", lambda e, xr=xr, t=t, eb=eb: e.dma_start(out=xr[:], in_=x[t * 128:(t + 1) * 128, eb * 512:(eb + 1) * 512]),
                      writes=[xrk], dma="xr_%s" % str(xrk))
                sc.op("dve", lambda e, xr=xr, tt=tt, eb=eb, bank=bank: e.scalar_tensor_tensor(
                    out=zt[tt][:, eb * 512:(eb + 1) * 512], in0=xr[:], scalar=ALPHA, in1=ps[bank][:], op0=ALU.mult, op1=ALU.add),
                    reads=[xrk, PSK[bank]], writes=[("z", tt)])
        for tt in range(4):
            t = g * 4 + tt
            if debug:
                sc.op("sp", lambda e, tt=tt, t=t: e.dma_start(out=dbg_z[t * 128:(t + 1) * 128, :], in_=zt[tt][:]),
                      reads=[("z", tt)], dma="dbg_st")
            _ln_tile(sc, zt[tt][:], ("z", tt), x1f[:], "x1f", stats, mv, smalls, lng, lnb)
            sc.op("act", lambda e: e.activation(out=x1b[:], in_=x1f[:], func=AF.Copy), reads=["x1f"], writes=["x1b"])
            sc.op("sp", lambda e, t=t: e.dma_start(out=X1B[t * 128:(t + 1) * 128, :], in_=x1b[:]), reads=["x1b"], writes=[("X1B", t)], dma="x1b_st")
            sc.op("act", lambda e: e.activation(out=ya_t[:], in_=x1f[:], func=AF.Copy, scale=ALPHA), reads=["x1f"], writes=["ya_t"])
            sc.op("sp", lambda e, t=t: e.dma_start(out=Y[t * 128:(t + 1) * 128, :], in_=ya_t[:]), reads=["ya_t"], writes=[("Y", t)], dma="ya_st")
            xTt = []
            for j in range(4):
                bank = j % 2
                xt_, xtk = get_tmp()
                xTt.append((xt_, xtk))

                def trr(e, j=j, bank=bank):
                    for i in range(4):
                        ins = e.transpose(out=ps[bank][:, i * 128:(i + 1) * 128], in_=x1f[:, (4 * j + i) * 128:(4 * j + i + 1) * 128], identity=identf[:])
                    return ins
                sc.op("pe", trr, reads=["x1f", "identf"], writes=[PSK[bank]])
                sc.op("act", lambda e, xt_=xt_, bank=bank: e.activation(out=xt_[:], in_=ps[bank][:], func=AF.Copy), reads=[PSK[bank]], writes=[xtk])

            def fr(e, xTt=xTt):
                for k in range(KD):
                    ins = e.matmul(ps[6][0:NE, 0:128], lhsT=wr_sb[:, k, :], rhs=xTt[k // 4][0][:, (k % 4) * 128:(k % 4 + 1) * 128],
                                   start=(k == 0), stop=(k == KD - 1))
                return ins
            sc.op("pe", fr, reads=["wr"] + [q[1] for q in xTt], writes=[PSK[6]])
            lg, lgk = get_tmp()
            sc.op("dve", lambda e, lg=lg: e.tensor_copy(out=lg[0:NE, 0:128], in_=ps[6][0:NE, 0:128]), reads=[PSK[6]], writes=[lgk])
            sc.op("sp", lambda e, lg=lg, t=t: e.dma_start(out=LOGT[:, t * 128:(t + 1) * 128], in_=lg[0:NE, 0:128]),
                  reads=[lgk], writes=["LOGT"], dma="lg_%s" % str(lgk))

    sc.barrier()
    es_p2.close()
    es_p3 = es.enter_context(ExitStack())
    scope[0] = es_p3
    lT = sb("lT", [NE, S])
    ex = sb("ex", [NE, S])
    aff = sb("aff", [NE, S])
    rsb = sb("rsb", [NE, 512])
    vals = sb("vals", [NE, CAP])
    idxu = sb("idxu", [NE, CAP], U32)
    idxf = sb("idxf", [NE, CAP])
    idxT = sb("idxT", [128, 2 * NE], U32)
    xgt = [sb("xgt%d" % i, [128, D], BF16) for i in range(3)]
    sc.op("sp", lambda e: e.dma_start(out=lT[:], in_=LOGT), reads=["LOGT"], writes=["lT"], dma="lT_ld")
    sc.op("act", lambda e: e.activation(out=ex[:], in_=lT[:], func=AF.Exp), reads=["lT"], writes=["ex"])
    for blk in range(4):
        sl = slice(blk * 512, (blk + 1) * 512)
        sc.op("pe", lambda e, blk=blk, sl=sl: e.matmul(ps[blk][0:NE, :], lhsT=onesf[0:NE, 0:NE], rhs=ex[:, sl], start=True, stop=True),
              reads=["ex", "onesf"], writes=[PSK[blk]])
        sc.op("dve", lambda e, blk=blk: e.reciprocal(out=rsb[:], in_=ps[blk][0:NE, :]), reads=[PSK[blk]], writes=["rsb"])
        sc.op("dve", lambda e, sl=sl: e.tensor_tensor(out=aff[:, sl], in0=ex[:, sl], in1=rsb[:], op=ALU.mult), reads=["ex", "rsb"], writes=["aff"])
    for it in range(CAP // 8):
        c8 = slice(it * 8, it * 8 + 8)
        sc.op("dve", lambda e, c8=c8: e.max(out=vals[:, c8], in_=aff[:]), reads=["aff"], writes=["vals"])
        sc.op("dve", lambda e, c8=c8: e.max_index(out=idxu[:, c8], in_max=vals[:, c8], in_values=aff[:]), reads=["aff", "vals"], writes=["idxu"])
        sc.op("dve", lambda e, c8=c8: e.match_replace(out=aff[:], in_to_replace=vals[:, c8], in_values=aff[:], imm_value=-1.0),
              reads=["vals", "aff"], writes=["aff"])
    sc.op("sp", lambda e: e.dma_start(out=idx_out, in_=idxu[:]), reads=["idxu"], dma="io_st")
    sc.op("sp", lambda e: e.dma_start(out=gate_out, in_=vals[:]), reads=["vals"], dma="io_st")
    sc.op("dve", lambda e: e.tensor_copy(out=idxf[:], in_=idxu[:]), reads=["idxu"], writes=["idxf"])
    for ct in range(2):
        sc.op("pe", lambda e, ct=ct: e.transpose(out=ps[4 + ct][:, 0:NE], in_=idxf[0:NE, ct * 128:(ct + 1) * 128], identity=identf[0:NE, 0:NE]),
              reads=["idxf", "identf"], writes=[PSK[4 + ct]])
        sc.op("dve", lambda e, ct=ct: e.tensor_copy(out=idxT[:, ct * NE:(ct + 1) * NE], in_=ps[4 + ct][:, 0:NE]), reads=[PSK[4 + ct]], writes=["idxT"])
    X1B_ALL = [("X1B", t) for t in range(NT)]
    n = 0
    for ex_ in range(NE):
        for ct in range(2):
            i = n % 3
            n += 1
            col = ct * NE + ex_
            sc.op("pool", lambda e, i=i, col=col: e.indirect_dma_start(
                out=xgt[i][:], out_offset=None, in_=X1B, in_offset=bass.IndirectOffsetOnAxis(ap=idxT[:, col:col + 1], axis=0)),
                reads=X1B_ALL + ["idxT"], writes=[("xgt", i)], dma="xgt%d" % i)
            r0 = ex_ * CAP + ct * 128
            sc.op("sp", lambda e, i=i, r0=r0: e.dma_start(out=xg_out[r0:r0 + 128, :], in_=xgt[i][:]), reads=[("xgt", i)], dma="xgst%d" % i)
    sc.final_wait("sp", ["ya_st", "io_st", "xgst0", "xgst1", "xgst2"] + (["dbg_st"] if debug else []))
    sc.emit()


def _rope_tables():
    f32 = np.float32
    pos = np.arange(S)
    row = (pos // 64).astype(f32)
    col = (pos % 64).astype(f32)
    invA = (f32(10000.0) ** (-np.arange(0, 64, 2, dtype=f32) / f32(64))).astype(f32)
    cA = np.ones((128, S), f32)
    sA = np.zeros((128, S), f32)
    permA = np.zeros((128, 128), f32)
    for p in range(128):
        posv = row if p < 64 else col
        j = p % 32
        ang = (posv * invA[j]).astype(f32)
        first = (p % 64) < 32
        cA[p] = np.cos(ang)
        sA[p] = -np.sin(ang) if first else np.sin(ang)
        partner = p + 32 if first else p - 32
        permA[partner, p] = 1.0
    invB = (f32(500000.0) ** (-np.arange(0, 32, 2, dtype=f32) / f32(32))).astype(f32)
    cB = np.ones((128, S), f32)
    sB = np.zeros((128, S), f32)
    permB = np.zeros((128, 128), f32)
    for p in range(128):
        if p < 32:
            j = p % 16
            ang = (pos.astype(f32) * invB[j]).astype(f32)
            first = p < 16
            cB[p] = np.cos(ang)
            sB[p] = -np.sin(ang) if first else np.sin(ang)
            partner = p + 16 if first else p - 16
        else:
            partner = p
        permB[partner, p] = 1.0
    return cA, sA, cB, sB, permA, permB


def _ln_tile(sc, zt_ap, zkey, dst, dkey, stats, mv, smalls, lng, lnb):
    for c in range(4):
        sc.op("dve", lambda e, c=c: e.bn_stats(out=stats[:, c, :], in_=zt_ap[:, c * 512:(c + 1) * 512]), reads=[zkey], writes=["stats"])
    sc.op("dve", lambda e: e.bn_aggr(out=mv[:, 0:2], in_=stats[:].rearrange("p a b -> p (a b)")), reads=["stats"], writes=["mv"])
    sc.op("act", lambda e: e.activation(out=mv[:, 2:3], in_=mv[:, 1:2], func=AF.Ln, bias=smalls[:, 6:7]), reads=["mv", "eps_ln"], writes=["mv2"])
    sc.op("act", lambda e: e.activation(out=mv[:, 2:3], in_=mv[:, 2:3], func=AF.Exp, scale=-0.5), reads=["mv2"], writes=["mv2"])
    sc.op("dve", lambda e: e.scalar_tensor_tensor(out=dst, in0=zt_ap, scalar=mv[:, 0:1], in1=lng[:], op0=ALU.subtract, op1=ALU.mult),
          reads=[zkey, "mv", "lng"], writes=[dkey])
    sc.op("dve", lambda e: e.scalar_tensor_tensor(out=dst, in0=dst, scalar=mv[:, 2:3], in1=lnb[:], op0=ALU.mult, op1=ALU.add),
          reads=[dkey, "mv2", "lnb"], writes=[dkey])


def build_p2():
    nc = bass.Bass("TRN2", target_bir_lowering=False)
    with ExitStack() as es:
        NTOK = NCORE * CAP
        xg = nc.dram_tensor("xg", [2 * NTOK, D], BF16, kind="ExternalInput").ap()
        wg = nc.dram_tensor("wg", [2, D, FF], F32, kind="ExternalInput").ap()
        wu = nc.dram_tensor("wu", [2, D, FF], F32, kind="ExternalInput").ap()
        wd = nc.dram_tensor("wd", [2, FF, D], F32, kind="ExternalInput").ap()
        identb_d = nc.dram_tensor("identb", [128, 128], BF16, kind="ExternalInput").ap()
        o = nc.dram_tensor("o", [2 * NTOK, D], F32, kind="ExternalOutput").ap()
        sc = Sched(nc, es)
        sb = lambda name, shape, dt=F32: es.enter_context(nc.sbuf_tensor("s_" + name, list(shape), dt))
        ps = [es.enter_context(nc.psum_tensor("ps%d" % i, [128, 512], F32)) for i in range(8)]
        PSK = [("ps", i) for i in range(8)]
        identb = sb("identb", [128, 128], BF16)
        sc.op("sp", lambda e: e.dma_start(out=identb[:], in_=identb_d), writes=["identb"], dma="c_identb")
        HT = 1024
        xst = [sb("xst%d" % i, [128, D], BF16) for i in range(2)]
        xgT = sb("xgT", [128, KD, HT], BF16)
        hidT = sb("hidT", [128, FF // 128, HT], BF16)
        NW = 4
        wbuf = [sb("wbuf%d" % i, [128, 16, 512], BF16) for i in range(NW)]
        NTMP = 4
        tmp = [sb("tmp%d" % i, [128, 512]) for i in range(NTMP)]
        NOS = 4
        ostg = [sb("ostg%d" % i, [128, 512]) for i in range(NOS)]
        st = {"w": 0, "tmp": 0, "os": 0, "n": 0}

        def load_w(src3):
            i = st["w"] % NW
            st["w"] += 1
            sc.op("pool", lambda e, i=i, src3=src3: e.dma_start(out=wbuf[i][:], in_=src3), writes=[("w", i)], dma="w%d" % i)
            return wbuf[i], ("w", i)

        for el in range(2):
            for hh in range(2):
                base = el * NTOK + hh * HT
                for tt in range(HT // 128):
                    xi = tt % 2
                    r0 = base + tt * 128
                    sc.op("sp", lambda e, xi=xi, r0=r0: e.dma_start(out=xst[xi][:], in_=xg[r0:r0 + 128, :]), writes=[("xst", xi)], dma="xst%d" % xi)
                    for jb in range(2):
                        bank = jb

                        def tr(e, xi=xi, jb=jb, bank=bank):
                            pv = ps[bank][:].bitcast(BF16)
                            for i in range(8):
                                ins = e.transpose(out=pv[:, i * 128:(i + 1) * 128], in_=xst[xi][:, (jb * 8 + i) * 128:(jb * 8 + i + 1) * 128], identity=identb[:])
                            return ins
                        sc.op("pe", tr, reads=[("xst", xi), "identb"], writes=[PSK[bank]])
                        dst = xgT[:, jb * 8:(jb + 1) * 8, tt * 128:(tt + 1) * 128]
                        if jb == 0:
                            sc.op("act", lambda e, dst=dst, bank=bank: e.activation(out=dst, in_=ps[bank][:].bitcast(BF16).rearrange("p (a b) -> p a b", a=8), func=AF.Copy),
                                  reads=[PSK[bank]], writes=["xgT"])
                        else:
                            sc.op("dve", lambda e, dst=dst, bank=bank: e.tensor_copy(out=dst, in_=ps[bank][:].bitcast(BF16).rearrange("p (a b) -> p a b", a=8)),
                                  reads=[PSK[bank]], writes=["xgT"])
                for fg in range(FF // 512):
                    wgb, wgk = load_w(wg[el, :, fg * 512:(fg + 1) * 512].rearrange("(k p) c -> p k c", p=128))
                    wub, wuk = load_w(wu[el, :, fg * 512:(fg + 1) * 512].rearrange("(k p) c -> p k c", p=128))
                    for fi in range(4):
                        kf = fg * 4 + fi
                        for tb in range(HT // 512):
                            par = st["n"] % 2
                            st["n"] += 1
                            gb, ub = 2 + par, 4 + par

                            def fgate(e, wgb=wgb, fi=fi, tb=tb, gb=gb):
                                for k in range(KD):
                                    ins = e.matmul(ps[gb][:], lhsT=wgb[:, k, fi * 128:(fi + 1) * 128], rhs=xgT[:, k, tb * 512:(tb + 1) * 512], start=(k == 0), stop=(k == KD - 1))
                                return ins

                            def fup(e, wub=wub, fi=fi, tb=tb, ub=ub):
                                for k in range(KD):
                                    ins = e.matmul(ps[ub][:], lhsT=wub[:, k, fi * 128:(fi + 1) * 128], rhs=xgT[:, k, tb * 512:(tb + 1) * 512], start=(k == 0), stop=(k == KD - 1))
                                return ins
                            sc.op("pe", fgate, reads=[wgk, "xgT"], writes=[PSK[gb]])
                            sc.op("pe", fup, reads=[wuk, "xgT"], writes=[PSK[ub]])
                            ti = st["tmp"] % NTMP
                            st["tmp"] += 1
                            sc.op("act", lambda e, ti=ti, gb=gb: e.activation(out=tmp[ti][:], in_=ps[gb][:], func=AF.Silu), reads=[PSK[gb]], writes=[("tmp", ti)])
                            sc.op("dve", lambda e, ti=ti, ub=ub, kf=kf, tb=tb: e.tensor_tensor(out=hidT[:, kf, tb * 512:(tb + 1) * 512], in0=tmp[ti][:], in1=ps[ub][:], op=ALU.mult),
                                  reads=[("tmp", ti), PSK[ub]], writes=[("hid", kf)])
                HID = [("hid", kf) for kf in range(FF // 128)]
                for db in range(4):
                    wd0, wd0k = load_w(wd[el, 0:2048, db * 512:(db + 1) * 512].rearrange("(k p) c -> p k c", p=128))
                    wd1, wd1k = load_w(wd[el, 2048:4096, db * 512:(db + 1) * 512].rearrange("(k p) c -> p k c", p=128))
                    for tt in range(HT // 128):
                        bank = (6, 7, 0, 1)[st["n"] % 4]
                        st["n"] += 1

                        def fdown(e, wd0=wd0, wd1=wd1, tt=tt, bank=bank):
                            for k in range(32):
                                wsrc = wd0 if k < 16 else wd1
                                ins = e.matmul(ps[bank][:], lhsT=hidT[:, k, tt * 128:(tt + 1) * 128], rhs=wsrc[:, k % 16, :], start=(k == 0), stop=(k == 31))
                            return ins
                        sc.op("pe", fdown, reads=[wd0k, wd1k] + HID, writes=[PSK[bank]])
                        oi = st["os"] % NOS
                        st["os"] += 1
                        if oi % 2 == 0:
                            sc.op("act", lambda e, oi=oi, bank=bank: e.activation(out=ostg[oi][:], in_=ps[bank][:], func=AF.Copy), reads=[PSK[bank]], writes=[("os", oi)])
                        else:
                            sc.op("dve", lambda e, oi=oi, bank=bank: e.tensor_copy(out=ostg[oi][:], in_=ps[bank][:]), reads=[PSK[bank]], writes=[("os", oi)])
                        r0 = base + tt * 128
                        sc.op("sp", lambda e, oi=oi, r0=r0, db=db: e.dma_start(out=o[r0:r0 + 128, db * 512:(db + 1) * 512], in_=ostg[oi][:]),
                              reads=[("os", oi)], dma="ost%d" % oi)
        sc.final_wait("sp", ["ost%d" % i for i in range(NOS)])
        sc.emit()
    return nc


def build_p3():
    nc = bass.Bass("TRN2", target_bir_lowering=False)
    with ExitStack() as es:
        y_in = nc.dram_tensor("y_in", [S, D], F32, kind="ExternalInput").ap()
        og = nc.dram_tensor("og", [NE * CAP, D], F32, kind="ExternalInput").ap()
        idxT_d = nc.dram_tensor("idxT", [128, 2 * NE], U32, kind="ExternalInput").ap()
        gateT_d = nc.dram_tensor("gateT", [128, 2 * NE], F32, kind="ExternalInput").ap()
        ln2g_d = nc.dram_tensor("ln2g", [128, D], F32, kind="ExternalInput").ap()
        ln2b_d = nc.dram_tensor("ln2b", [128, D], F32, kind="ExternalInput").ap()
        out = nc.dram_tensor("out", [S, D], F32, kind="ExternalOutput").ap()
        YW = nc.dram_tensor("YW", [S, D], F32).ap()
        sc = Sched(nc, es)
        sb = lambda name, shape, dt=F32: es.enter_context(nc.sbuf_tensor("s_" + name, list(shape), dt))
        idxT = sb("idxT", [128, 2 * NE], U32)
        gateT = sb("gateT", [128, 2 * NE])
        lng = sb("lng", [128, D])
        lnb = sb("lnb", [128, D])
        smalls = sb("smalls", [128, 8])
        stats = sb("stats", [128, 4, 6])
        mv = sb("mv", [128, 8])
        ot = [sb("ot%d" % i, [128, D]) for i in range(3)]
        zt = [sb("z%d" % i, [128, D]) for i in range(2)]
        x1f = sb("x1f", [128, D])
        for dst, src, key in ((idxT, idxT_d, "idxT"), (gateT, gateT_d, "gateT"), (lng, ln2g_d, "lng"), (lnb, ln2b_d, "lnb")):
            sc.op("sp", lambda e, dst=dst, src=src: e.dma_start(out=dst[:], in_=src), writes=[key], dma="c_" + key)
        sc.op("pool", lambda e: e.memset(smalls[:, 6:7], LN_EPS), writes=["eps_ln"])
        sc.op("pool", lambda e: e.dma_start(out=YW, in_=y_in), writes=["YW"], dma="yw")
        for j in range(2 * NE):
            i = j % 3
            sc.op("sp", lambda e, i=i, j=j: e.dma_start(out=ot[i][:], in_=og[j * 128:(j + 1) * 128, :]), writes=[("ot", i)], dma="ot%d" % i)
            sc.op("dve", lambda e, i=i, j=j: e.tensor_scalar(out=ot[i][:], in0=ot[i][:], scalar1=gateT[:, j:j + 1], scalar2=None, op0=ALU.mult),
                  reads=[("ot", i), "gateT"], writes=[("ot", i)])
            sc.op("pool", lambda e, i=i, j=j: e.indirect_dma_start(
                out=YW, out_offset=bass.IndirectOffsetOnAxis(ap=idxT[:, j:j + 1], axis=0), in_=ot[i][:], in_offset=None, compute_op=ALU.add),
                reads=[("ot", i), "idxT"], writes=["YW"], dma="yw")
        for t in range(NT):
            zi = t % 2
            sc.op("sp", lambda e, zi=zi, t=t: e.dma_start(out=zt[zi][:], in_=YW[t * 128:(t + 1) * 128, :]), reads=["YW"], writes=[("z", zi)], dma="yld%d" % zi)
            _ln_tile(sc, zt[zi][:], ("z", zi), x1f[:], "x1f", stats, mv, smalls, lng, lnb)
            sc.op("sp", lambda e, t=t: e.dma_start(out=out[t * 128:(t + 1) * 128, :], in_=x1f[:]), reads=["x1f"], dma="out_st")
        sc.final_wait("sp", ["out_st"])
        sc.emit()
    return nc


_NC_CACHE = {}


def kernel(x, w_in, b_gate, a_q_norm, a_k_norm, b_lambda, b_subln, w_a_proj, w_b_proj,
           w_o, ln1_g, ln1_b, w_router, w_gate, w_up, w_down, ln2_g, ln2_b):
    f32 = np.float32
    A = lambda a: np.ascontiguousarray(np.asarray(a, dtype=f32))
    x = A(x)
    for name, fn in (("p1", build_p1), ("p2", build_p2), ("p3", build_p3)):
        if name not in _NC_CACHE:
            _NC_CACHE[name] = fn()
    cA, sA, cB, sB, permA, permB = _rope_tables()
    rep = lambda v: np.ascontiguousarray(np.broadcast_to(A(v).reshape(1, -1), (128, A(v).size)))
    identb = np.eye(128, dtype=f32).astype(ml_dtypes.bfloat16)
    shared = {
        "w_in": A(w_in[0]),
        "bgT": np.ascontiguousarray(A(b_gate[0]).reshape(32, 128).T),
        "gq": A(a_q_norm[0]).reshape(128, 1),
        "gk": A(a_k_norm[0]).reshape(128, 1),
        "lamb": rep(b_lambda[0]),
        "subln": np.ascontiguousarray(A(b_subln[0]).reshape(2, 128).T),
        "w_a": A(w_a_proj[0]), "w_b": A(w_b_proj[0]), "w_o": A(w_o[0]),
        "ln1g": rep(ln1_g[0]), "ln1b": rep(ln1_b[0]),
        "w_r": A(w_router[0]),
        "ropeA_c": cA, "ropeA_s": sA, "ropeB_c": cB, "ropeB_s": sB,
        "permA": permA, "permB": permB,
        "identf": np.eye(128, dtype=f32), "identb": identb,
    }
    cores = list(range(NCORE))
    r1 = run_bass_kernel_spmd(_NC_CACHE["p1"], [dict(shared, x=x[c]) for c in cores], core_ids=cores).results
    xg_all = np.stack([np.asarray(r["xg_out"]) for r in r1], 0).reshape(NCORE, NCORE, 2, CAP, D)
    xg_p2 = np.ascontiguousarray(xg_all.transpose(1, 2, 0, 3, 4)).reshape(NCORE, 2 * NCORE * CAP, D)
    del xg_all
    wg_all, wu_all, wd_all = A(w_gate[0]), A(w_up[0]), A(w_down[0])
    r2 = run_bass_kernel_spmd(_NC_CACHE["p2"], [
        {"xg": xg_p2[c], "wg": wg_all[2 * c:2 * c + 2], "wu": wu_all[2 * c:2 * c + 2], "wd": wd_all[2 * c:2 * c + 2], "identb": identb}
        for c in cores], core_ids=cores).results
    o_all = np.stack([np.asarray(r["o"], dtype=f32) for r in r2], 0).reshape(NCORE, 2, NCORE, CAP, D)
    og_p3 = np.ascontiguousarray(o_all.transpose(2, 0, 1, 3, 4)).reshape(NCORE, NE * CAP, D)
    del o_all
    lay = lambda a: np.ascontiguousarray(np.asarray(a).reshape(NE, 2, 128).transpose(2, 0, 1).reshape(128, 2 * NE))
    ln2g, ln2b = rep(ln2_g[0]), rep(ln2_b[0])
    r3 = run_bass_kernel_spmd(_NC_CACHE["p3"], [
        {"y_in": np.asarray(r1[c]["y_out"], dtype=f32), "og": og_p3[c],
         "idxT": lay(np.asarray(r1[c]["idx_out"]).astype(np.uint32)), "gateT": lay(np.asarray(r1[c]["gate_out"], dtype=f32)),
         "ln2g": ln2g, "ln2b": ln2b} for c in cores], core_ids=cores).results
    return np.stack([np.asarray(r["out"], dtype=f32) for r in r3], axis=0)
```
